# Optimizing a Trainium2 kernel written in Bass

```python
import math
import jax, jax.numpy as jnp
from jax import lax
import numpy as np

D_MODEL = 2048
BATCH = 2
SEQ = 8192
DEPTH = 4

N_A_LAYERS = DEPTH // 2
N_B_LAYERS = DEPTH - N_A_LAYERS
NORM_EPS = 1e-6
GDN_HEADS = D_MODEL // 128
GDN_HEAD_DIM = 128
GDN_WIDTH = GDN_HEADS * GDN_HEAD_DIM
GDN_IN_WIDTH = 4 * GDN_WIDTH + 2 * GDN_HEADS
CONV_K = 4
CHUNK = 64
DIFF_HEADS = D_MODEL // 256
DIFF_HEAD_DIM = 128
DIFF_Q_WIDTH = DIFF_HEADS * 2 * DIFF_HEAD_DIM
DIFF_V_WIDTH = DIFF_HEADS * 2 * DIFF_HEAD_DIM
Q_BLOCK = 128
ROPE_THETA = 10000.0
PEER_HEADS = 8
PEER_QUERY_DIM = 256
PEER_HALF = PEER_QUERY_DIM // 2
N_KEYS = 128
N_EXPERTS = N_KEYS * N_KEYS
PEER_TOPK = 16
PEER_TOKEN_BLOCK = 128

kernel_name = 'yoco_gdn_diffattn_peer_adaln'


def rmsnorm(x, g):
    xf = x.astype(jnp.float32)
    y = xf * lax.rsqrt(jnp.mean(xf * xf, axis=-1, keepdims=True) + NORM_EPS)
    return (y * g.astype(jnp.float32)).astype(x.dtype)


def modulate(h, shift, scale):
    return h * (1 + scale[:, None, :]) + shift[:, None, :]


def adaln(c, w, b):
    return jax.nn.silu(c) @ w + b


def l2norm(x):
    xf = x.astype(jnp.float32)
    return xf * lax.rsqrt(jnp.sum(xf * xf, axis=-1, keepdims=True) + NORM_EPS)


def rope_tables(positions):
    inv_freq = ROPE_THETA ** (-jnp.arange(0, DIFF_HEAD_DIM, 2, dtype=jnp.float32) / DIFF_HEAD_DIM)
    ang = positions.astype(jnp.float32)[..., None] * inv_freq
    return jnp.cos(ang), jnp.sin(ang)


def apply_rope(x, cos, sin):
    cos = cos[:, :, None, None, :]
    sin = sin[:, :, None, None, :]
    xf = x.astype(jnp.float32)
    x1, x2 = jnp.split(xf, 2, axis=-1)
    return jnp.concatenate([x1 * cos - x2 * sin, x2 * cos + x1 * sin], axis=-1)


def causal_short_conv(x, w):
    s = x.shape[1]
    xp = jnp.pad(x, ((0, 0), (CONV_K - 1, 0), (0, 0)))
    out = xp[:, 0:s, :] * w[0]
    for i in range(1, CONV_K):
        out = out + xp[:, i:i + s, :] * w[i]
    return out


def chunk_gated_delta_rule(q, k, v, g, beta):
    b, s, nh, dk = q.shape
    dv = v.shape[-1]
    n = s // CHUNK

    def to_chunks(t):
        t = jnp.moveaxis(t.astype(jnp.float32), 2, 1)
        return t.reshape((b, nh, n, CHUNK) + t.shape[3:])

    q, k, v, g, beta = [to_chunks(t) for t in (q, k, v, g, beta)]
    G = jnp.cumsum(g, axis=-1)
    diff = G[..., :, None] - G[..., None, :]
    idx = jnp.arange(CHUNK)
    strict = idx[:, None] > idx[None, :]
    incl = idx[:, None] >= idx[None, :]
    kb = k * beta[..., None]
    L = jnp.einsum('bhnid,bhnjd->bhnij', kb, k) * jnp.exp(jnp.where(strict, diff, -jnp.inf))
    T = L + jnp.eye(CHUNK, dtype=jnp.float32)
    w = lax.linalg.triangular_solve(T, kb * jnp.exp(G)[..., None],
                                    left_side=True, lower=True, unit_diagonal=True)
    u_tilde = lax.linalg.triangular_solve(T, v * beta[..., None],
                                          left_side=True, lower=True, unit_diagonal=True)
    a_qk = jnp.einsum('bhnid,bhnjd->bhnij', q, k) * jnp.exp(jnp.where(incl, diff, -jnp.inf))
    q_g = q * jnp.exp(G)[..., None]
    k_d = k * jnp.exp(G[..., -1:] - G)[..., None]
    g_last = jnp.exp(G[..., -1])

    def step(state, inp):
        w_c, u_c, a_c, q_c, k_c, gl = inp
        u_new = u_c - jnp.einsum('bhck,bhkv->bhcv', w_c, state)
        o = jnp.einsum('bhck,bhkv->bhcv', q_c, state) + jnp.einsum('bhij,bhjv->bhiv', a_c, u_new)
        state = gl[..., None, None] * state + jnp.einsum('bhck,bhcv->bhkv', k_c, u_new)
        return state, o

    xs = tuple(jnp.moveaxis(t, 2, 0) for t in (w, u_tilde, a_qk, q_g, k_d, g_last))
    state0 = jnp.zeros((b, nh, dk, dv), jnp.float32)
    _, o = lax.scan(step, state0, xs)
    o = jnp.moveaxis(o, 0, 2).reshape(b, nh, s, dv)
    return jnp.moveaxis(o, 1, 2)


def gated_deltanet(h, w_in, conv_w, a_log, dt_bias, o_gain, w_out):
    b, s, _ = h.shape
    W = GDN_WIDTH
    proj = h @ w_in
    qkv = jax.nn.silu(causal_short_conv(proj[..., :3 * W], conv_w))
    z = proj[..., 3 * W:4 * W]
    a = proj[..., 4 * W:4 * W + GDN_HEADS]
    beta_logit = proj[..., 4 * W + GDN_HEADS:]
    q, k, v = [t.reshape(b, s, GDN_HEADS, GDN_HEAD_DIM) for t in jnp.split(qkv, 3, axis=-1)]
    q = l2norm(q) * GDN_HEAD_DIM ** -0.5
    k = l2norm(k)
    g = -jnp.exp(a_log.astype(jnp.float32)) * jax.nn.softplus(a.astype(jnp.float32) + dt_bias.astype(jnp.float32))
    beta = jax.nn.sigmoid(beta_logit.astype(jnp.float32))
    o = chunk_gated_delta_rule(q, k, v, g, beta)
    o = rmsnorm(o, o_gain) * jax.nn.silu(z.reshape(b, s, GDN_HEADS, GDN_HEAD_DIM).astype(jnp.float32))
    return o.reshape(b, s, W).astype(h.dtype) @ w_out


def shared_kv(x, c, norm_g, ada_w, ada_b, w_kv, cos, sin):
    b, s, _ = x.shape
    shift, scale = jnp.split(adaln(c, ada_w, ada_b), 2, axis=-1)
    h = modulate(rmsnorm(x, norm_g), shift, scale)
    kv = h @ w_kv
    k = apply_rope(kv[..., :DIFF_Q_WIDTH].reshape(b, s, DIFF_HEADS, 2, DIFF_HEAD_DIM), cos, sin)
    v = kv[..., DIFF_Q_WIDTH:].reshape(b, s, DIFF_HEADS, 2 * DIFF_HEAD_DIM).astype(jnp.float32)
    return jnp.transpose(k, (0, 2, 3, 1, 4)), jnp.transpose(v, (0, 2, 1, 3))


def diff_attention(h, k_sh, v_sh, cos, sin, w_q, lam_p, subln_g, w_out, lambda_init):
    b, s, _ = h.shape
    d = DIFF_HEAD_DIM
    q = apply_rope((h @ w_q).reshape(b, s, DIFF_HEADS, 2, d), cos, sin) * d ** -0.5
    lp = lam_p.astype(jnp.float32)
    lam = jnp.exp(jnp.sum(lp[0] * lp[1])) - jnp.exp(jnp.sum(lp[2] * lp[3])) + lambda_init
    nb = s // Q_BLOCK
    q_blocks = jnp.transpose(q, (0, 2, 3, 1, 4)).reshape(b, DIFF_HEADS, 2, nb, Q_BLOCK, d)
    q_blocks = jnp.moveaxis(q_blocks, 3, 0)
    starts = jnp.arange(nb, dtype=jnp.int32) * Q_BLOCK
    k_pos = jnp.arange(s, dtype=jnp.int32)

    def block(args):
        qb, start = args
        scores = jnp.einsum('bhcqd,bhckd->bhcqk', qb, k_sh)
        mask = k_pos[None, :] <= (start + jnp.arange(Q_BLOCK, dtype=jnp.int32))[:, None]
        p = jax.nn.softmax(jnp.where(mask, scores, -jnp.inf), axis=-1)
        attn = p[:, :, 0] - lam * p[:, :, 1]
        return jnp.einsum('bhqk,bhkv->bhqv', attn, v_sh)

    o = lax.map(block, (q_blocks, starts))
    o = jnp.moveaxis(o, 0, 2).reshape(b, DIFF_HEADS, s, 2 * d)
    o = jnp.moveaxis(o, 1, 2)
    o = rmsnorm(o, subln_g) * (1.0 - lambda_init)
    return o.reshape(b, s, DIFF_V_WIDTH).astype(h.dtype) @ w_out


def peer_ffn(h, w_q, sub_keys, u_tab, v_tab):
    b, s, dm = h.shape
    q = (h @ w_q).reshape(b, s, PEER_HEADS, 2, PEER_HALF).astype(jnp.float32)
    sc = jnp.einsum('bspjd,jnd->bspjn', q, sub_keys.astype(jnp.float32))
    s1, i1 = lax.top_k(sc[..., 0, :], PEER_TOPK)
    s2, i2 = lax.top_k(sc[..., 1, :], PEER_TOPK)
    n_cand = PEER_TOPK * PEER_TOPK
    cand = (s1[..., :, None] + s2[..., None, :]).reshape(b, s, PEER_HEADS, n_cand)
    cidx = (i1[..., :, None] * N_KEYS + i2[..., None, :]).reshape(b, s, PEER_HEADS, n_cand)
    best, pos = lax.top_k(cand, PEER_TOPK)
    idx = jnp.take_along_axis(cidx, pos, axis=-1)
    gate = jax.nn.softmax(best, axis=-1)
    n_sel = PEER_HEADS * PEER_TOPK
    nblk = (b * s) // PEER_TOKEN_BLOCK
    hb = h.reshape(nblk, PEER_TOKEN_BLOCK, dm)
    ib = idx.reshape(nblk, PEER_TOKEN_BLOCK, n_sel)
    gb = gate.reshape(nblk, PEER_TOKEN_BLOCK, n_sel).astype(h.dtype)

    def block(args):
        hx, ix, gx = args
        act = jax.nn.gelu(jnp.einsum('td,ted->te', hx, u_tab[ix]), approximate=False)
        return jnp.einsum('te,ted->td', gx * act, v_tab[ix])

    out = lax.map(block, (hb, ib, gb))
    return out.reshape(b, s, dm)


def setup_inputs(seed: int = 0) -> dict:
    key = jax.random.key(seed)
    ks = jax.random.split(key, 32)
    f32 = jnp.float32

    def nrm(k, shape, scale):
        return jax.random.normal(k, shape, f32) * scale

    def gain(k, shape):
        return 1.0 + 0.02 * jax.random.normal(k, shape, f32)

    D = D_MODEL
    x = nrm(ks[0], (BATCH, SEQ, D), 1.0)
    c = nrm(ks[1], (BATCH, D), 1.0)
    positions = (jax.random.randint(ks[2], (BATCH, 1), 0, 4096, dtype=jnp.int32)
                 + jnp.arange(SEQ, dtype=jnp.int32)[None, :])
    ada_w = nrm(ks[3], (DEPTH, D, 6 * D), 0.5 * D ** -0.5)
    ada_b = nrm(ks[4], (DEPTH, 6 * D), 0.02)
    norm_mix_g = gain(ks[5], (DEPTH, D))
    norm_ffn_g = gain(ks[6], (DEPTH, D))
    gdn_w_in = nrm(ks[7], (N_A_LAYERS, D, GDN_IN_WIDTH), D ** -0.5)
    gdn_conv_w = nrm(ks[8], (N_A_LAYERS, CONV_K, 3 * GDN_WIDTH), CONV_K ** -0.5)
    gdn_a_log = jnp.log(jax.random.uniform(ks[9], (N_A_LAYERS, GDN_HEADS), f32, 1.0, 16.0))
    dt = jnp.exp(jax.random.uniform(ks[10], (N_A_LAYERS, GDN_HEADS), f32, math.log(0.001), math.log(0.1)))
    gdn_dt_bias = dt + jnp.log(-jnp.expm1(-dt))
    gdn_o_gain = gain(ks[11], (N_A_LAYERS, GDN_HEAD_DIM))
    gdn_w_out = nrm(ks[12], (N_A_LAYERS, GDN_WIDTH, D), GDN_WIDTH ** -0.5)
    kv_norm_g = gain(ks[13], (D,))
    kv_ada_w = nrm(ks[14], (D, 2 * D), 0.5 * D ** -0.5)
    kv_ada_b = nrm(ks[15], (2 * D,), 0.02)
    kv_w = nrm(ks[16], (D, DIFF_Q_WIDTH + DIFF_V_WIDTH), D ** -0.5)
    diff_w_q = nrm(ks[17], (N_B_LAYERS, D, DIFF_Q_WIDTH), D ** -0.5)
    diff_lambda = nrm(ks[18], (N_B_LAYERS, 4, DIFF_HEAD_DIM), 0.1)
    diff_subln_g = gain(ks[19], (N_B_LAYERS, 2 * DIFF_HEAD_DIM))
    diff_w_out = nrm(ks[20], (N_B_LAYERS, DIFF_V_WIDTH, D), DIFF_V_WIDTH ** -0.5)
    peer_w_q = nrm(ks[21], (DEPTH, D, PEER_HEADS * PEER_QUERY_DIM), D ** -0.5)
    peer_sub_keys = nrm(ks[22], (DEPTH, 2, N_KEYS, PEER_HALF), PEER_HALF ** -0.5)
    peer_u = nrm(ks[23], (DEPTH, N_EXPERTS, D), D ** -0.5)
    peer_v = nrm(ks[24], (DEPTH, N_EXPERTS, D), PEER_HEADS ** -0.5)
    final_g = gain(ks[25], (D,))
    return {'x': x, 'c': c, 'positions': positions,
            'ada_w': ada_w, 'ada_b': ada_b, 'norm_mix_g': norm_mix_g, 'norm_ffn_g': norm_ffn_g,
            'gdn_w_in': gdn_w_in, 'gdn_conv_w': gdn_conv_w, 'gdn_a_log': gdn_a_log,
            'gdn_dt_bias': gdn_dt_bias, 'gdn_o_gain': gdn_o_gain, 'gdn_w_out': gdn_w_out,
            'kv_norm_g': kv_norm_g, 'kv_ada_w': kv_ada_w, 'kv_ada_b': kv_ada_b, 'kv_w': kv_w,
            'diff_w_q': diff_w_q, 'diff_lambda': diff_lambda, 'diff_subln_g': diff_subln_g,
            'diff_w_out': diff_w_out,
            'peer_w_q': peer_w_q, 'peer_sub_keys': peer_sub_keys, 'peer_u': peer_u, 'peer_v': peer_v,
            'final_g': final_g}


def reference(x, c, positions, ada_w, ada_b, norm_mix_g, norm_ffn_g,
              gdn_w_in, gdn_conv_w, gdn_a_log, gdn_dt_bias, gdn_o_gain, gdn_w_out,
              kv_norm_g, kv_ada_w, kv_ada_b, kv_w,
              diff_w_q, diff_lambda, diff_subln_g, diff_w_out,
              peer_w_q, peer_sub_keys, peer_u, peer_v, final_g):
    cos, sin = rope_tables(positions)
    k_sh, v_sh = None, None
    for l in range(DEPTH):
        sh1, sc1, gt1, sh2, sc2, gt2 = jnp.split(adaln(c, ada_w[l], ada_b[l]), 6, axis=-1)
        h = modulate(rmsnorm(x, norm_mix_g[l]), sh1, sc1)
        if l < N_A_LAYERS:
            y = gated_deltanet(h, gdn_w_in[l], gdn_conv_w[l], gdn_a_log[l], gdn_dt_bias[l],
                               gdn_o_gain[l], gdn_w_out[l])
        else:
            j = l - N_A_LAYERS
            lambda_init = 0.8 - 0.6 * math.exp(-0.3 * l)
            y = diff_attention(h, k_sh, v_sh, cos, sin, diff_w_q[j], diff_lambda[j],
                               diff_subln_g[j], diff_w_out[j], lambda_init)
        x = x + gt1[:, None, :] * y
        h = modulate(rmsnorm(x, norm_ffn_g[l]), sh2, sc2)
        x = x + gt2[:, None, :] * peer_ffn(h, peer_w_q[l], peer_sub_keys[l], peer_u[l], peer_v[l])
        if l == N_A_LAYERS - 1:
            k_sh, v_sh = shared_kv(x, c, kv_norm_g, kv_ada_w, kv_ada_b, kv_w, cos, sin)
    return rmsnorm(x, final_g)
```

```python
from contextlib import ExitStack
import numpy as np
import concourse.bass as bass
import concourse.mybir as mybir
from concourse.bass_utils import run_bass_kernel_spmd

F32 = mybir.dt.float32
AF = mybir.ActivationFunctionType
ALU = mybir.AluOpType
AX = mybir.AxisListType


class Prog:
    ENG = ("pe", "act", "dve", "pool", "sp")

    def __init__(self):
        self.nc = bass.Bass("TRN2", target_bir_lowering=False)
        self.stack = ExitStack()
        self.ops = {e: [] for e in self.ENG}
        self.cnt = {}
        self.sems = {}
        self.waited = {e: {} for e in self.ENG}
        self.last_w = {}
        self.readers = {}
        self.nm = 0

    def sem(self, name):
        if name not in self.sems:
            self.sems[name] = self.stack.enter_context(self.nc.semaphore(name))
            self.cnt[name] = 0
        return self.sems[name]

    def sbuf(self, name, shape, dtype=F32):
        return self.stack.enter_context(self.nc.sbuf_tensor(name, list(shape), dtype))

    def psum(self, name, shape, dtype=F32):
        return self.stack.enter_context(self.nc.psum_tensor(name, list(shape), dtype))

    def dram(self, name, shape, dtype=F32, kind="Internal"):
        return self.nc.dram_tensor(name, list(shape), dtype, kind=kind).ap()

    def _deps(self, reads, writes):
        d = {}

        def add(ev):
            if ev is None:
                return
            s, v = ev
            if d.get(s, 0) < v:
                d[s] = v
        for k in reads:
            add(self.last_w.get(k))
        for k in writes:
            add(self.last_w.get(k))
            for s, v in self.readers.get(k, {}).items():
                add((s, v))
        return d

    def _commit(self, ev, reads, writes):
        for k in writes:
            self.last_w[k] = ev
            self.readers[k] = {}
        for k in reads:
            r = self.readers.setdefault(k, {})
            if r.get(ev[0], 0) < ev[1]:
                r[ev[0]] = ev[1]

    def _waits(self, eng, deps, own):
        w = []
        for s, v in deps.items():
            if s == own and eng == "pe":
                continue
            if self.waited[eng].get(s, 0) >= v:
                continue
            self.waited[eng][s] = v
            w.append((s, v))
        return w

    @staticmethod
    def _keys(aps):
        return [a.name for a in aps if a is not None and hasattr(a, "name")]

    def op(self, eng, fn, reads, writes):
        own = "e_" + eng
        self.sem(own)
        rk, wk = self._keys(reads), self._keys(writes)
        deps = self._deps(rk, wk)
        waits = self._waits(eng, deps, own)
        self.cnt[own] += 1
        ev = (own, self.cnt[own])
        self.ops[eng].append((waits, fn, own, 1))
        self._commit(ev, rk, wk)

    def dma(self, out, in_, eng="sp", **kw):
        rk, wk = self._keys([in_]), self._keys([out])
        sname = "d_" + (wk[0] if wk else rk[0])
        self.sem(sname)
        deps = self._deps(rk, wk)
        if self.cnt[sname] > 0:
            if deps.get(sname, 0) < self.cnt[sname]:
                deps[sname] = self.cnt[sname]
        waits = self._waits(eng, deps, None)
        self.cnt[sname] += 16
        ev = (sname, self.cnt[sname])
        self.ops[eng].append((waits, lambda e: e.dma_start(out=out, in_=in_, **kw), sname, 16))
        self._commit(ev, rk, wk)

    def mm(self, out, lhsT, rhs, start=True, stop=True):
        self.op("pe", lambda e: e.matmul(out, lhsT, rhs, start=start, stop=stop), [lhsT, rhs], [out])

    def transpose(self, out, in_, ident):
        self.op("pe", lambda e: e.transpose(out, in_, ident), [in_, ident], [out])

    def act(self, out, in_, func, bias=None, scale=None, accum_out=None):
        kw = {}
        if bias is not None:
            kw["bias"] = bias
        if scale is not None:
            kw["scale"] = scale
        if accum_out is not None:
            kw["accum_out"] = accum_out
        self.op("act", lambda e: e.activation(out, in_, func, **kw), [in_, bias, scale], [out, accum_out])

    def tt(self, out, in0, in1, op, eng="dve"):
        self.op(eng, lambda e: e.tensor_tensor(out, in0, in1, op), [in0, in1], [out])

    def ts(self, out, in0, s1, s2, op0, op1=None, eng="dve", accum_out=None):
        kw = {}
        if accum_out is not None:
            kw["accum_out"] = accum_out
        if op1 is None:
            self.op(eng, lambda e: e.tensor_scalar(out, in0, s1, None, op0, **kw), [in0, s1], [out, accum_out])
        else:
            self.op(eng, lambda e: e.tensor_scalar(out, in0, s1, s2, op0, op1, **kw), [in0, s1, s2], [out, accum_out])

    def stt(self, out, in0, scalar, in1, op0, op1, eng="dve"):
        self.op(eng, lambda e: e.scalar_tensor_tensor(out, in0, scalar, in1, op0, op1), [in0, scalar, in1], [out])

    def copy(self, out, in_, eng="dve"):
        if eng == "act":
            self.op("act", lambda e: e.copy(out, in_), [in_], [out])
        else:
            self.op(eng, lambda e: e.tensor_copy(out, in_), [in_], [out])

    def memset(self, out, val, eng="dve"):
        self.op(eng, lambda e: e.memset(out, val), [], [out])

    def reduce(self, out, in_, op, axis=AX.X, eng="dve"):
        self.op(eng, lambda e: e.tensor_reduce(out, in_, axis, op), [in_], [out])

    def vmax8(self, out, in_):
        self.op("dve", lambda e: e.max(out, in_), [in_], [out])

    def match_replace(self, out, to_replace, values, imm):
        self.op("dve", lambda e: e.match_replace(out, to_replace, values, imm), [to_replace, values], [out])

    def recip(self, out, in_):
        self.op("dve", lambda e: e.reciprocal(out, in_), [in_], [out])

    def build(self):
        nc = self.nc
        fin = []
        for s, v in self.cnt.items():
            if v > 0 and self.waited["sp"].get(s, 0) < v:
                fin.append((s, v))
        sems = self.sems
        ops = self.ops

        def emit(e, lst, extra=()):
            for waits, fn, s, inc in lst:
                for ws, wv in waits:
                    e.wait_ge(sems[ws], wv)
                fn(e).then_inc(sems[s], inc)
            for ws, wv in extra:
                e.wait_ge(sems[ws], wv)

        with nc.Block() as block:
            @block.tensor
            def _(e):
                emit(e, ops["pe"])

            @block.scalar
            def _(e):
                emit(e, ops["act"])

            @block.vector
            def _(e):
                emit(e, ops["dve"])

            @block.gpsimd
            def _(e):
                emit(e, ops["pool"])

            @block.sync
            def _(e):
                emit(e, ops["sp"], fin)
        self.stack.close()
        return nc

    def ninstr(self):
        return {e: len(v) for e, v in self.ops.items()}


def run(P, in_maps, trace=False):
    nc = P.build()
    return run_bass_kernel_spmd(nc, in_maps, core_ids=list(range(len(in_maps))), trace=trace)


def build_ada(NCOL):
    P = Prog()
    nc = P.nc
    cT = P.dram("cT", [128, 16, 2], kind="ExternalInput")
    W = P.dram("W", [2048, NCOL], kind="ExternalInput")
    b = P.dram("b", [1, NCOL], kind="ExternalInput")
    out = P.dram("out", [2, NCOL], kind="ExternalOutput")
    c_sb = P.sbuf("c_sb", [128, 16, 2])
    sc = P.sbuf("sc", [128, 16, 2])
    b_sb = P.sbuf("b_sb", [2, NCOL])
    o_sb = P.sbuf("o_sb", [2, NCOL])
    wb = [P.sbuf(f"wb{i}", [128, 16, 512]) for i in range(2)]
    ps = [P.psum(f"ps{i}", [2, 512]) for i in range(2)]
    P.dma(c_sb[:], cT)
    P.dma(b_sb[0:1, :], b)
    P.dma(b_sb[1:2, :], b)
    P.act(sc[:], c_sb[:], AF.Silu)
    Wv = W.rearrange("(k p) n -> p k n", p=128)
    nt = NCOL // 512
    for t in range(nt):
        w = wb[t % 2]
        P.dma(w[:], Wv[:, :, t * 512:(t + 1) * 512], eng="sp" if t % 2 == 0 else "pool")
        for k in range(16):
            P.mm(ps[t % 2][:], sc[:, k, :], w[:, k, :], start=(k == 0), stop=(k == 15))
        P.tt(o_sb[:, t * 512:(t + 1) * 512], ps[t % 2][:], b_sb[:, t * 512:(t + 1) * 512], ALU.add)
    P.dma(out, o_sb[:])
    return P


def build_mid(T, NK, final=False):
    NE = NK * NK
    P = Prog()
    xT = P.dram("xT", [D, T], kind="ExternalInput")
    oT = P.dram("oT", [D, T], kind="ExternalInput")
    w_out = P.dram("w_out", [D, D], kind="ExternalInput")
    w_q = P.dram("w_q", [D, D], kind="ExternalInput")
    vecs = P.dram("vecs", [128, 6, KC], kind="ExternalInput")
    keysT = P.dram("keysT", [128, 2, NK], kind="ExternalInput")
    UT = P.dram("UT", [D, NE], kind="ExternalInput")
    Vt = P.dram("Vt", [NE, D], kind="ExternalInput")
    ident_d = P.dram("ident", [128, 128], kind="ExternalInput")
    x2T = P.dram("x2T", [D, T], kind="ExternalOutput")

    ST = 256
    NSUB = ST // 128
    EI = min(16, NK)
    GW = EI * NK
    NG = NE // GW
    EB = 256
    NB = GW // EB
    I1B = EB // NK

    vec = P.sbuf("vec", [128, 6, KC])
    A2 = P.sbuf("A2", [128, KC])
    keys = P.sbuf("keys", [128, 2, NK])
    ident = P.sbuf("identsb", [128, 128])
    ones = P.sbuf("ones", [128, 128])
    xs = P.sbuf("xs", [128, KC, ST])
    os_ = P.sbuf("os", [128, KC, ST])
    wblk = [P.sbuf(f"wblk{i}", [128, KC, 128]) for i in range(2)]
    S = [P.sbuf(f"S{i}", [128, 8, 2, NK]) for i in range(NSUB)]
    qTc = [P.sbuf(f"qTc{i}", [128, ST]) for i in range(2)]
    sq = [P.sbuf(f"sq{i}", [128, ST]) for i in range(2)]
    rstd = P.sbuf("rstd", [128, ST])
    t1 = P.sbuf("t1", [128, 16]); t2 = P.sbuf("t2", [128, 16]); bb = P.sbuf("bb", [128, 16]); eb = P.sbuf("eb", [128, 16])
    tmpk = P.sbuf("tmpk", [128, NK]); cand = P.sbuf("cand", [128, 16, 16]); tmpc = P.sbuf("tmpc", [128, 256])
    tau = [P.sbuf(f"tau{i}", [128, 8]) for i in range(NSUB)]
    negm = [P.sbuf(f"negm{i}", [128, 8]) for i in range(NSUB)]
    Z = [P.sbuf(f"Z{i}", [128, 8]) for i in range(NSUB)]
    invZ = [P.sbuf(f"invZ{i}", [128, 8]) for i in range(NSUB)]
    cbuf = P.sbuf("cbuf", [128, EI, NK]); ebuf = P.sbuf("ebuf", [128, EI, NK]); gbuf = P.sbuf("gbuf", [128, EI, NK])
    gate = P.sbuf("gate", [128, GW])
    UTb = [P.sbuf(f"UTb{i}", [128, KC, EB]) for i in range(2)]
    Vb = [P.sbuf(f"Vb{i}", [128, EB // 128, D]) for i in range(2)]
    ga = [P.sbuf(f"ga{i}", [128, EB]) for i in range(2)]
    gaT = [P.sbuf(f"gaT{i}", [128, EB // 128, 128]) for i in range(2)]
    acc = P.sbuf("acc", [128, D])
    psA = [P.psum(f"psA{i}", [128, 512]) for i in range(2)]
    psB = [P.psum(f"psB{i}", [128, 512]) for i in range(2)]
    psO = P.psum("psO", [128, D])

    P.dma(vec[:], vecs)
    P.dma(keys[:], keysT)
    P.dma(ident[:], ident_d)
    P.memset(ones[:], 1.0)
    P.ts(A2[:], vec[:, 2, :], 1.0, None, ALU.add)
    P.tt(A2[:], A2[:], vec[:, 4, :], ALU.mult)
    GT1, SH2, GT2, FG = 0, 1, 3, 5

    xTv = xT.rearrange("(k p) t -> p k t", p=128)
    oTv = oT.rearrange("(k p) t -> p k t", p=128)
    x2v = x2T.rearrange("(k p) t -> p k t", p=128)
    wov = w_out.rearrange("(k p) n -> p k n", p=128)
    wqv = w_q.rearrange("(k p) n -> p k n", p=128)
    UTv = UT.rearrange("(k p) e -> p k e", p=128)
    wi = 0

    def rms(dst_fn):
        for j in range(KC):
            P.act(sq[j % 2][:], xs[:, j, :], AF.Square)
            P.mm(psB[0][:, :ST], ones[:], sq[j % 2][:], start=(j == 0), stop=(j == KC - 1))
        P.act(rstd[:], psB[0][:, :ST], AF.Sqrt, bias=EPS, scale=1.0 / D)
        P.recip(rstd[:], rstd[:])

    for st in range(T // ST):
        t0 = st * ST
        P.dma(xs[:], xTv[:, :, t0:t0 + ST])
        P.dma(os_[:], oTv[:, :, t0:t0 + ST])
        for j in range(KC):
            w = wblk[wi % 2]; wi += 1
            P.dma(w[:], wov[:, :, j * 128:(j + 1) * 128])
            pa = psA[j % 2]
            for k in range(KC):
                P.mm(pa[:, :ST], w[:, k, :], os_[:, k, :], start=(k == 0), stop=(k == KC - 1))
            P.stt(xs[:, j, :], pa[:, :ST], vec[:, GT1, j:j + 1], xs[:, j, :], ALU.mult, ALU.add)
        rms(None)
        for j in range(KC):
            P.stt(sq[j % 2][:], xs[:, j, :], A2[:, j:j + 1], rstd[:], ALU.mult, ALU.mult)
            P.act(os_[:, j, :], sq[j % 2][:], AF.Identity, bias=vec[:, SH2, j:j + 1])
        for c in range(16):
            p, jj = c // 2, c % 2
            w = wblk[wi % 2]; wi += 1
            P.dma(w[:], wqv[:, :, c * 128:(c + 1) * 128])
            pa = psA[c % 2]
            for k in range(KC):
                P.mm(pa[:, :ST], w[:, k, :], os_[:, k, :], start=(k == 0), stop=(k == KC - 1))
            q = qTc[c % 2]
            P.copy(q[:], pa[:, :ST], eng="act")
            for sub in range(NSUB):
                pb = psB[sub % 2]
                P.mm(pb[:, :NK], q[:, sub * 128:(sub + 1) * 128], keys[:, jj, :])
                P.copy(S[sub][:, p, jj, :], pb[:, :NK])
        for sub in range(NSUB):
            for p in range(8):
                for (tk, jj) in ((t1, 0), (t2, 1)):
                    s = S[sub][:, p, jj, :]
                    P.vmax8(tk[:, 0:8], s)
                    P.match_replace(tmpk[:], tk[:, 0:8], s, -1e30)
                    P.vmax8(tk[:, 8:16], tmpk[:])
                P.tt(cand[:], t1[:].unsqueeze(2).to_broadcast([128, 16, 16]),
                     t2[:].unsqueeze(1).to_broadcast([128, 16, 16]), ALU.add)
                cf = cand[:].rearrange("p a b -> p (a b)")
                P.vmax8(bb[:, 0:8], cf)
                P.match_replace(tmpc[:], bb[:, 0:8], cf, -1e30)
                P.vmax8(bb[:, 8:16], tmpc[:])
                P.copy(tau[sub][:, p:p + 1], bb[:, 15:16])
                P.ts(negm[sub][:, p:p + 1], bb[:, 0:1], -1.0, None, ALU.mult)
                P.act(eb[:], bb[:], AF.Exp, bias=negm[sub][:, p:p + 1])
                P.reduce(Z[sub][:, p:p + 1], eb[:], ALU.add)
            P.recip(invZ[sub][:], Z[sub][:])
        bi = 0
        for sub in range(NSUB):
            tsl = slice(sub * 128, (sub + 1) * 128)
            for g in range(NG):
                i0 = g * EI
                for p in range(8):
                    P.tt(cbuf[:], S[sub][:, p, 0, i0:i0 + EI].unsqueeze(2).to_broadcast([128, EI, NK]),
                         S[sub][:, p, 1, :].unsqueeze(1).to_broadcast([128, EI, NK]), ALU.add, eng="pool")
                    P.act(ebuf[:], cbuf[:], AF.Exp, bias=negm[sub][:, p:p + 1])
                    P.stt(gbuf[:], cbuf[:], tau[sub][:, p:p + 1], ebuf[:], ALU.is_ge, ALU.mult)
                    gf = gbuf[:].rearrange("p a b -> p (a b)")
                    if p == 0:
                        P.ts(gate[:], gf, invZ[sub][:, p:p + 1], None, ALU.mult)
                    else:
                        P.stt(gate[:], gf, invZ[sub][:, p:p + 1], gate[:], ALU.mult, ALU.add)
                for b in range(NB):
                    e0 = g * GW + b * EB
                    ub = UTb[bi % 2]; vb = Vb[bi % 2]; gg = ga[bi % 2]; gt = gaT[bi % 2]
                    pa = psA[bi % 2]; pt = psB[bi % 2]
                    first = (g == 0 and b == 0); last = (g == NG - 1 and b == NB - 1)
                    bi += 1
                    P.dma(ub[:], UTv[:, :, e0:e0 + EB])
                    P.dma(vb[:], Vt[e0:e0 + EB, :].rearrange("(c p) d -> p c d", p=128))
                    for k in range(KC):
                        P.mm(pa[:, :EB], os_[:, k, tsl], ub[:, k, :], start=(k == 0), stop=(k == KC - 1))
                    P.act(gg[:], pa[:, :EB], AF.Gelu)
                    P.tt(gg[:], gg[:], gate[:, b * EB:(b + 1) * EB], ALU.mult)
                    for c in range(EB // 128):
                        P.transpose(pt[:, c * 128:(c + 1) * 128], gg[:, c * 128:(c + 1) * 128], ident[:])
                    P.copy(gt[:].rearrange("p c t -> p (c t)"), pt[:, :EB], eng="pool" if False else "dve")
                    for dt in range(4):
                        for c in range(EB // 128):
                            P.mm(psO[:, dt * 512:(dt + 1) * 512], gt[:, c, :], vb[:, c, dt * 512:(dt + 1) * 512],
                                 start=(first and c == 0), stop=(last and c == EB // 128 - 1))
            P.copy(acc[:], psO[:], eng="act")
            for j in range(KC):
                pa = psA[j % 2]
                P.transpose(pa[:, :128], acc[:, j * 128:(j + 1) * 128], ident[:])
                P.stt(xs[:, j, tsl], pa[:, :128], vec[:, GT2, j:j + 1], xs[:, j, tsl], ALU.mult, ALU.add)
        if final:
            rms(None)
            for j in range(KC):
                P.stt(xs[:, j, :], xs[:, j, :], vec[:, FG, j:j + 1], rstd[:], ALU.mult, ALU.mult)
        P.dma(x2v[:, :, t0:t0 + ST], xs[:])
    return P


NH = 4

def build_gdn(S):
    P = Prog()
    xT = P.dram("xT", [D, S], kind="ExternalInput")
    wq = P.dram("w_qkvz", [D, 16 * 128], kind="ExternalInput")
    wab = P.dram("w_ab", [D, 8], kind="ExternalInput")
    convw = P.dram("convw", [128, 12, 4], kind="ExternalInput")
    vecs = P.dram("vecs", [128, 3, KC], kind="ExternalInput")
    hp = P.dram("hp", [128, 2, NH], kind="ExternalInput")
    ogain = P.dram("ogain", [128, 1], kind="ExternalInput")
    consts = P.dram("consts", [128, 4, 128], kind="ExternalInput")
    ogT = P.dram("ogT", [NH * 128, S], kind="ExternalOutput")

    TT = 512
    NSUB = TT // 128
    vec = P.sbuf("vec", [128, 3, KC]); A1 = P.sbuf("A1", [128, KC])
    hps = P.sbuf("hps", [128, 2, NH]); negA = P.sbuf("negA", [128, NH])
    og = P.sbuf("og", [128, 1]); cw = P.sbuf("cw", [128, 12, 4])
    cst = P.sbuf("cst", [128, 4, 128])
    ident, triI, triS, ones = cst[:, 0, :], cst[:, 1, :], cst[:, 2, :], cst[:, 3, :]
    wabs = P.sbuf("wabs", [128, KC, 8])
    xs = P.sbuf("xs", [128, KC, TT]); hs = P.sbuf("hs", [128, KC, TT])
    wblk = [P.sbuf(f"wblk{i}", [128, KC, 128]) for i in range(2)]
    sq = [P.sbuf(f"sq{i}", [128, TT]) for i in range(2)]
    rstd = P.sbuf("rstd", [128, TT])
    pr = [P.sbuf(f"pr{c}", [128, 3 + TT]) for c in range(16)]
    cv = [P.sbuf(f"cv{c}", [128, TT]) for c in range(12)]
    oT = [P.sbuf(f"oT{h}", [128, TT]) for h in range(NH)]
    ab = [P.sbuf(f"ab{s}", [128, 8]) for s in range(NSUB)]
    Sst = [P.sbuf(f"Sst{h}", [128, 128]) for h in range(NH)]

    def hb(name, shape=(128, 128)):
        return [P.sbuf(f"{name}{h}", list(shape)) for h in range(NH)]
    gcol = P.sbuf("gcol", [128, NH]); beta = P.sbuf("beta", [128, NH]); nbeta = P.sbuf("nbeta", [128, NH])
    l4 = P.sbuf("l4", [128, NH]); p4 = P.sbuf("p4", [128, NH]); m4 = P.sbuf("m4", [128, NH])
    u4 = P.sbuf("u4", [128, NH]); Gc = P.sbuf("Gc", [128, NH]); eG = P.sbuf("eG", [128, NH]); neG = P.sbuf("neG", [128, NH])
    dcol = P.sbuf("dcol", [128, NH]); gl = P.sbuf("gl", [128, NH]); Glast = P.sbuf("Glast", [128, NH])
    gb = hb("gb"); arg = hb("arg"); E = hb("E"); DTi = hb("DTi"); DTs = hb("DTs"); EGb = hb("EGb")
    AqkT = hb("AqkT"); Pm = [hb("Pm0"), hb("Pm1")]; Qm = [hb("Qm0"), hb("Qm1")]; X = hb("X")
    Ktok = hb("Ktok"); Vtok = hb("Vtok"); Rp = hb("Rp"); un = hb("un"); QgT = hb("QgT")
    psBig = [P.psum(f"psBig{i}", [128, 512]) for i in range(2)]
    psHb = [P.psum(f"psHb{i}", [128, 256]) for i in range(6)]
    psH = [[psHb[(h * 3 + i) // 2][:, ((h * 3 + i) % 2) * 128:((h * 3 + i) % 2) * 128 + 128] for i in range(3)] for h in range(NH)]

    P.dma(vec[:], vecs); P.dma(hps[:], hp); P.dma(og[:], ogain); P.dma(cw[:], convw); P.dma(cst[:], consts)
    P.dma(wabs[:], wab.rearrange("(k p) n -> p k n", p=128))
    P.ts(A1[:], vec[:, 1, :], 1.0, None, ALU.add)
    P.tt(A1[:], A1[:], vec[:, 2, :], ALU.mult)
    P.act(negA[:], hps[:, 0, :], AF.Exp)
    P.ts(negA[:], negA[:], -1.0, None, ALU.mult)
    for h in range(NH):
        P.memset(Sst[h][:], 0.0)
    for c in range(16):
        P.memset(pr[c][:, 0:3], 0.0)

    xTv = xT.rearrange("(k p) t -> p k t", p=128)
    wqv = wq.rearrange("(k p) n -> p k n", p=128)
    wi = 0
    for tl in range(S // TT):
        t0 = tl * TT
        P.dma(xs[:], xTv[:, :, t0:t0 + TT])
        for j in range(KC):
            P.act(sq[j % 2][:], xs[:, j, :], AF.Square)
            P.mm(psBig[0][:], ones, sq[j % 2][:], start=(j == 0), stop=(j == KC - 1))
        P.act(rstd[:], psBig[0][:], AF.Sqrt, bias=EPS, scale=1.0 / D)
        P.recip(rstd[:], rstd[:])
        for j in range(KC):
            P.stt(sq[j % 2][:], xs[:, j, :], A1[:, j:j + 1], rstd[:], ALU.mult, ALU.mult)
            P.act(hs[:, j, :], sq[j % 2][:], AF.Identity, bias=vec[:, 0, j:j + 1])
        for c in range(16):
            h, kind = c // 4, c % 4
            w = wblk[wi % 2]; wi += 1
            P.dma(w[:], wqv[:, :, c * 128:(c + 1) * 128])
            pb = psBig[c % 2]
            for k in range(KC):
                P.mm(pb[:], w[:, k, :], hs[:, k, :], start=(k == 0), stop=(k == KC - 1))
            if kind < 3 and tl > 0:
                P.copy(pr[c][:, 0:3], pr[c][:, TT:TT + 3], eng="pool")
            P.copy(pr[c][:, 3:3 + TT], pb[:], eng="act")
        for s in range(NSUB):
            pb = psBig[s % 2]
            for k in range(KC):
                P.mm(pb[:, 0:8], hs[:, k, s * 128:(s + 1) * 128], wabs[:, k, :], start=(k == 0), stop=(k == KC - 1))
            P.copy(ab[s][:], pb[:, 0:8])
        for h in range(NH):
            for kind in range(3):
                c = h * 4 + kind; ci = h * 3 + kind
                o = cv[ci]
                P.ts(o[:], pr[c][:, 0:TT], cw[:, ci, 0:1], None, ALU.mult)
                for i in range(1, 4):
                    P.stt(o[:], pr[c][:, i:i + TT], cw[:, ci, i:i + 1], o[:], ALU.mult, ALU.add)
                P.act(o[:], o[:], AF.Silu)
                if kind < 2:
                    P.act(sq[kind][:], o[:], AF.Square)
                    pb = psBig[kind]
                    P.mm(pb[:], ones, sq[kind][:])
                    P.act(sq[kind][:], pb[:], AF.Sqrt, bias=EPS)
                    P.recip(sq[kind][:], sq[kind][:])
                    if kind == 0:
                        P.stt(o[:], o[:], 128.0 ** -0.5, sq[kind][:], ALU.mult, ALU.mult)
                    else:
                        P.tt(o[:], o[:], sq[kind][:], ALU.mult)
        for s in range(NSUB):
            cs = slice(s * 128, (s + 1) * 128)
            P.tt(u4[:], ab[s][:, 0:NH], hps[:, 1, :], ALU.add)
            P.act(u4[:], u4[:], AF.Exp)
            P.act(l4[:], u4[:], AF.Ln, bias=1.0)
            P.ts(p4[:], u4[:], -0.2, 0.25, ALU.mult, ALU.add)
            for cst_ in (1.0 / 3, 0.5, 1.0):
                P.tt(p4[:], p4[:], u4[:], ALU.mult)
                P.ts(p4[:], p4[:], -1.0, cst_, ALU.mult, ALU.add)
            P.tt(p4[:], p4[:], u4[:], ALU.mult)
            P.ts(m4[:], u4[:], 0.1, None, ALU.is_lt)
            P.tt(p4[:], p4[:], l4[:], ALU.subtract)
            P.tt(p4[:], p4[:], m4[:], ALU.mult)
            P.tt(u4[:], l4[:], p4[:], ALU.add)
            P.tt(gcol[:], u4[:], negA[:], ALU.mult)
            P.act(beta[:], ab[s][:, NH:2 * NH], AF.Sigmoid)
            P.ts(nbeta[:], beta[:], -1.0, None, ALU.mult)
            P.mm(psBig[0][:, 0:NH], triI, gcol[:])
            P.copy(Gc[:], psBig[0][:, 0:NH])
            P.act(eG[:], Gc[:], AF.Exp)
            P.ts(neG[:], eG[:], -1.0, None, ALU.mult)
            for h in range(NH):
                qT, kT, vT = cv[h * 3][:, cs], cv[h * 3 + 1][:, cs], cv[h * 3 + 2][:, cs]
                p0, p1, p2 = psH[h]
                P.ts(gb[h][:], ones, gcol[:, h:h + 1], None, ALU.mult)
                P.mm(p0[:], gb[h][:], triI)
                P.copy(Glast[:, h:h + 1], p0[:, 127:128])
                P.ts(arg[h][:], p0[:], Gc[:, h:h + 1], 0.0, ALU.subtract, ALU.min)
                P.act(E[h][:], arg[h][:], AF.Exp)
                P.act(EGb[h][:], p0[:], AF.Exp)
                P.tt(DTi[h][:], E[h][:], triI, ALU.mult)
                P.tt(DTs[h][:], E[h][:], triS, ALU.mult)
                P.act(dcol[:, h:h + 1], Gc[:, h:h + 1], AF.Exp, scale=-1.0, bias=Glast[:, h:h + 1])
                P.act(gl[:, h:h + 1], Glast[:, h:h + 1], AF.Exp)
                P.tt(QgT[h][:], qT, EGb[h][:], ALU.mult)
                P.mm(p1[:], kT, kT)
                M = Pm[0][h]
                P.stt(M[:], p1[:], nbeta[:, h:h + 1], DTs[h][:], ALU.mult, ALU.mult)
                P.mm(p2[:], kT, qT)
                P.tt(AqkT[h][:], p2[:], DTi[h][:], ALU.mult)
                P.transpose(p1[:], M[:], ident)
                P.copy(Qm[0][h][:], p1[:], eng="act")
                P.tt(X[h][:], M[:], ident, ALU.add)
                P.transpose(p2[:], kT, ident)
                P.ts(Ktok[h][:], p2[:], dcol[:, h:h + 1], None, ALU.mult)
                P.transpose(p0[:], vT, ident)
                P.copy(Vtok[h][:], p0[:], eng="act")
            for lv in range(6):
                a, b_ = lv % 2, (lv + 1) % 2
                for h in range(NH):
                    p0, p1, p2 = psH[h]
                    P.mm(p1[:], Pm[a][h][:], Qm[a][h][:])
                    P.copy(Qm[b_][h][:], p1[:], eng="act")
                    if lv < 5:
                        P.mm(p0[:], Qm[a][h][:], Pm[a][h][:])
                        P.copy(Pm[b_][h][:], p0[:])
                    P.mm(p2[:], Qm[b_][h][:], X[h][:])
                    P.tt(X[h][:], X[h][:], p2[:], ALU.add)
            for h in range(NH):
                kT = cv[h * 3 + 1][:, cs]
                p0, p1, p2 = psH[h]
                P.mm(p0[:], kT, Sst[h][:])
                P.stt(Rp[h][:], p0[:], neG[:, h:h + 1], Vtok[h][:], ALU.mult, ALU.add)
            for h in range(NH):
                p0, p1, p2 = psH[h]
                P.mm(p1[:], X[h][:], Rp[h][:])
                P.ts(un[h][:], p1[:], beta[:, h:h + 1], None, ALU.mult)
            for h in range(NH):
                p0, p1, p2 = psH[h]
                P.mm(p2[:], Sst[h][:], QgT[h][:], start=True, stop=False)
                P.mm(p2[:], un[h][:], AqkT[h][:], start=False, stop=True)
                P.copy(oT[h][:, cs], p2[:], eng="act")
                P.mm(p0[:], Ktok[h][:], un[h][:])
                P.stt(Sst[h][:], Sst[h][:], gl[:, h:h + 1], p0[:], ALU.mult, ALU.add)
        for h in range(NH):
            zc = pr[h * 4 + 3]
            P.act(sq[0][:], oT[h][:], AF.Square)
            pb = psBig[h % 2]
            P.mm(pb[:], ones, sq[0][:])
            P.act(sq[1][:], pb[:], AF.Sqrt, bias=EPS, scale=1.0 / 128)
            P.recip(sq[1][:], sq[1][:])
            P.stt(oT[h][:], oT[h][:], og[:, 0:1], sq[1][:], ALU.mult, ALU.mult)
            P.act(sq[0][:], zc[:, 3:3 + TT], AF.Silu)
            P.tt(oT[h][:], oT[h][:], sq[0][:], ALU.mult)
            P.dma(ogT[h * 128:(h + 1) * 128, t0:t0 + TT], oT[h][:])
    return P


def gdn_consts():
    i = np.arange(128)
    ident = np.eye(128, dtype=np.float32)
    triI = (i[:, None] <= i[None, :]).astype(np.float32)
    triS = (i[:, None] < i[None, :]).astype(np.float32)
    ones = np.ones((128, 128), np.float32)
    return np.ascontiguousarray(np.stack([ident, triI, triS, ones], axis=1))

I32 = mybir.dt.int32

def build_proj(T, NR, NV, qscale):
    N = NR + NV
    P = Prog()
    xT = P.dram("xT", [D, T], kind="ExternalInput")
    W = P.dram("W", [D, N], kind="ExternalInput")
    Wsw = P.dram("Wsw", [D, NR], kind="ExternalInput")
    vecs = P.dram("vecs", [128, 3, KC], kind="ExternalInput")
    posb = P.dram("posb", [128, T], I32, kind="ExternalInput")
    invf = P.dram("invf", [128, 2], kind="ExternalInput")
    outT = P.dram("outT", [N, T], kind="ExternalOutput")
    TT = 512
    vec = P.sbuf("vec", [128, 3, KC]); A1 = P.sbuf("A1", [128, KC]); ones = P.sbuf("ones", [128, 128])
    ivf = P.sbuf("ivf", [128, 2]); pi_ = P.sbuf("pi_", [128, TT], I32); ang = P.sbuf("ang", [128, TT])
    ki = P.sbuf("ki", [128, TT], I32); fx = P.sbuf("fx", [128, TT]); cosF = P.sbuf("cosF", [128, TT]); sinS = P.sbuf("sinS", [128, TT]); tmp = P.sbuf("tmp", [128, TT])
    xs = P.sbuf("xs", [128, KC, TT]); hs = P.sbuf("hs", [128, KC, TT])
    sq = [P.sbuf(f"sq{i}", [128, TT]) for i in range(2)]
    rstd = P.sbuf("rstd", [128, TT])
    wblk = [P.sbuf(f"wblk{i}", [128, KC, 128]) for i in range(2)]
    wsblk = [P.sbuf(f"wsblk{i}", [128, KC, 128]) for i in range(2)]
    ob = [P.sbuf(f"ob{i}", [128, TT]) for i in range(2)]
    ps = [P.psum(f"ps{i}", [128, 512]) for i in range(2)]
    ps2 = [P.psum(f"ps2{i}", [128, 512]) for i in range(2)]
    P.dma(vec[:], vecs); P.dma(ivf[:], invf); P.memset(ones[:], 1.0)
    P.ts(A1[:], vec[:, 1, :], 1.0, None, ALU.add)
    P.tt(A1[:], A1[:], vec[:, 2, :], ALU.mult)
    xTv = xT.rearrange("(k p) t -> p k t", p=128)
    Wv = W.rearrange("(k p) n -> p k n", p=128)
    Wsv = Wsw.rearrange("(k p) n -> p k n", p=128)
    TWO_PI = 2 * math.pi
    for tl in range(T // TT):
        t0 = tl * TT
        P.dma(xs[:], xTv[:, :, t0:t0 + TT])
        P.dma(pi_[:], posb[:, t0:t0 + TT])
        P.copy(ang[:], pi_[:])
        P.ts(ang[:], ang[:], ivf[:, 0:1], None, ALU.mult)
        def sin_of(dst, src_ang, scale_ops):
            P.ts(tmp[:], src_ang, 1.0 / TWO_PI, None, ALU.mult)
            P.copy(ki[:], tmp[:])
            P.copy(tmp[:], ki[:])
            P.stt(tmp[:], tmp[:], -TWO_PI, src_ang, ALU.mult, ALU.add)
            P.ts(fx[:], tmp[:], math.pi, TWO_PI, ALU.is_gt, ALU.mult)
            P.tt(tmp[:], tmp[:], fx[:], ALU.subtract)
            P.ts(fx[:], tmp[:], -math.pi, TWO_PI, ALU.is_lt, ALU.mult)
            P.tt(tmp[:], tmp[:], fx[:], ALU.add)
            P.act(dst, tmp[:], AF.Sin)
        sin_of(sinS[:], ang[:], None)
        P.ts(sinS[:], sinS[:], ivf[:, 1:2], None, ALU.mult)
        P.ts(sinS[:], sinS[:], qscale, None, ALU.mult)
        P.ts(ang[:], ang[:], math.pi / 2, None, ALU.add)
        sin_of(cosF[:], ang[:], None)
        P.ts(cosF[:], cosF[:], qscale, None, ALU.mult)
        for j in range(KC):
            P.act(sq[j % 2][:], xs[:, j, :], AF.Square)
            P.mm(ps[0][:], ones[:], sq[j % 2][:], start=(j == 0), stop=(j == KC - 1))
        P.act(rstd[:], ps[0][:], AF.Sqrt, bias=EPS, scale=1.0 / D)
        P.recip(rstd[:], rstd[:])
        for j in range(KC):
            P.stt(sq[j % 2][:], xs[:, j, :], A1[:, j:j + 1], rstd[:], ALU.mult, ALU.mult)
            P.act(hs[:, j, :], sq[j % 2][:], AF.Identity, bias=vec[:, 0, j:j + 1])
        for c in range(N // 128):
            w = wblk[c % 2]
            P.dma(w[:], Wv[:, :, c * 128:(c + 1) * 128])
            pa = ps[c % 2]
            for k in range(KC):
                P.mm(pa[:], w[:, k, :], hs[:, k, :], start=(k == 0), stop=(k == KC - 1))
            o = ob[c % 2]
            if c * 128 < NR:
                ws = wsblk[c % 2]
                P.dma(ws[:], Wsv[:, :, c * 128:(c + 1) * 128])
                pb = ps2[c % 2]
                for k in range(KC):
                    P.mm(pb[:], ws[:, k, :], hs[:, k, :], start=(k == 0), stop=(k == KC - 1))
                P.tt(o[:], pa[:], cosF[:], ALU.mult)
                P.tt(tmp[:], pb[:], sinS[:], ALU.mult)
                P.tt(o[:], o[:], tmp[:], ALU.add)
            else:
                P.copy(o[:], pa[:], eng="act")
            P.dma(outT[c * 128:(c + 1) * 128, t0:t0 + TT], o[:])
    return P


def build_attn(S, lambda_init):
    NHD = 2
    NB = S // 128
    P = Prog()
    qT = P.dram("qT", [NHD, 2, 128, S], kind="ExternalInput")
    kT = P.dram("kT", [NHD, 2, 128, S], kind="ExternalInput")
    v = P.dram("v", [S, NHD, 256], kind="ExternalInput")
    lamp = P.dram("lamp", [128, 4, 128], kind="ExternalInput")
    gsub = P.dram("gsub", [128, 256], kind="ExternalInput")
    consts = P.dram("consts", [128, 2, 128], kind="ExternalInput")
    out = P.dram("out", [S, NHD, 256], kind="ExternalOutput")
    cst = P.sbuf("cst", [128, 2, 128]); ident, cmask = cst[:, 0, :], cst[:, 1, :]
    lp = P.sbuf("lp", [128, 4, 128]); lt = P.sbuf("lt", [128, 2, 128]); l2 = P.sbuf("l2", [128, 2]); neglam = P.sbuf("neglam", [128, 1])
    gs = P.sbuf("gs", [128, 256])
    ks = [P.sbuf(f"ks{c}", [128, S]) for c in range(2)]
    vs = P.sbuf("vs", [128, NB, 256])
    Pb = [P.sbuf(f"Pb{c}", [128, S]) for c in range(2)]
    qs = [[P.sbuf(f"qs{c}_{i}", [128, 128]) for i in range(2)] for c in range(2)]
    PT = [P.sbuf(f"PT{i}", [128, 128]) for i in range(2)]
    m = P.sbuf("m", [128, 2]); negm = P.sbuf("negm", [128, 2]); Z = P.sbuf("Z", [128, 2]); iZ = P.sbuf("iZ", [128, 2]); nl1 = P.sbuf("nl1", [128, 1])
    ob = [P.sbuf(f"ob{i}", [128, 256]) for i in range(2)]; osq = P.sbuf("osq", [128, 256]); ss = P.sbuf("ss", [128, 1])
    psS = [P.psum(f"psS{i}", [128, 512]) for i in range(2)]
    psT = [P.psum(f"psT{i}", [128, 128]) for i in range(2)]
    psO = [P.psum(f"psO{i}", [128, 256]) for i in range(2)]
    P.dma(cst[:], consts); P.dma(lp[:], lamp); P.dma(gs[:], gsub)
    P.tt(lt[:, 0, :], lp[:, 0, :], lp[:, 1, :], ALU.mult)
    P.tt(lt[:, 1, :], lp[:, 2, :], lp[:, 3, :], ALU.mult)
    P.reduce(l2[:], lt[:], ALU.add)
    P.act(l2[:], l2[:], AF.Exp)
    P.tt(neglam[:], l2[:, 1:2], l2[:, 0:1], ALU.subtract)
    P.ts(neglam[:], neglam[:], -lambda_init, None, ALU.add)
    it = 0
    for hd in range(NHD):
        for c in range(2):
            P.dma(ks[c][:], kT[hd, c])
        P.dma(vs[:], v[:, hd, :].rearrange("(n p) d -> p n d", p=128))
        for qb in range(NB):
            nk = (qb + 1) * 128
            par = it % 2; it += 1
            for c in range(2):
                q = qs[c][par]
                P.dma(q[:], qT[hd, c, :, qb * 128:(qb + 1) * 128])
                for kt in range((nk + 511) // 512):
                    w = min(512, nk - kt * 512)
                    pp = psS[(kt + c) % 2]
                    P.mm(pp[:, :w], q[:], ks[c][:, kt * 512:kt * 512 + w])
                    P.copy(Pb[c][:, kt * 512:kt * 512 + w], pp[:, :w], eng="act" if kt % 2 else "dve")
                P.tt(Pb[c][:, nk - 128:nk], Pb[c][:, nk - 128:nk], cmask, ALU.add)
                P.reduce(m[:, c:c + 1], Pb[c][:, :nk], ALU.max)
                P.ts(negm[:, c:c + 1], m[:, c:c + 1], -1.0, None, ALU.mult)
                P.act(Pb[c][:, :nk], Pb[c][:, :nk], AF.Exp, bias=negm[:, c:c + 1])
                P.reduce(Z[:, c:c + 1], Pb[c][:, :nk], ALU.add)
            P.recip(iZ[:], Z[:])
            P.tt(nl1[:], iZ[:, 1:2], neglam[:], ALU.mult)
            P.ts(Pb[0][:, :nk], Pb[0][:, :nk], iZ[:, 0:1], None, ALU.mult)
            P.stt(Pb[0][:, :nk], Pb[1][:, :nk], nl1[:, 0:1], Pb[0][:, :nk], ALU.mult, ALU.add)
            po = psO[par]
            for kb in range(qb + 1):
                pt = psT[kb % 2]; ptS = PT[kb % 2]
                P.transpose(pt[:], Pb[0][:, kb * 128:(kb + 1) * 128], ident)
                P.copy(ptS[:], pt[:], eng="act" if kb % 2 else "dve")
                P.mm(po[:], ptS[:], vs[:, kb, :], start=(kb == 0), stop=(kb == qb))
            o = ob[par]
            P.copy(o[:], po[:], eng="act")
            P.tt(osq[:], o[:], o[:], ALU.mult)
            P.reduce(ss[:], osq[:], ALU.add)
            P.act(ss[:], ss[:], AF.Sqrt, bias=EPS, scale=1.0 / 256)
            P.recip(ss[:], ss[:])
            P.stt(o[:], o[:], ss[:, 0:1], gs[:], ALU.mult, ALU.mult)
            P.ts(o[:], o[:], 1.0 - lambda_init, None, ALU.mult)
            P.dma(out[qb * 128:(qb + 1) * 128, hd, :], o[:])
    return P


def attn_consts():
    i = np.arange(128)
    ident = np.eye(128, dtype=np.float32)
    cm = np.where(i[None, :] > i[:, None], -1e30, 0.0).astype(np.float32)
    return np.ascontiguousarray(np.stack([ident, cm], axis=1))


def rope_consts():
    inv = (10000.0 ** (-np.arange(0, 128, 2, dtype=np.float32) / 128)).astype(np.float32)
    invf = np.concatenate([inv, inv])
    sign = np.concatenate([-np.ones(64, np.float32), np.ones(64, np.float32)])
    return np.ascontiguousarray(np.stack([invf, sign], axis=1))


def swap_cols(W, NR):
    Wr = W[:, :NR].reshape(W.shape[0], NR // 128, 2, 64)
    return np.ascontiguousarray(Wr[:, :, ::-1, :].reshape(W.shape[0], NR))


import math
D = 2048
KC = 16
EPS = 1e-6
NH = 4
_CACHE = {}


def _prog(key, fn):
    if key not in _CACHE:
        _CACHE[key] = fn().build()
    return _CACHE[key]


def _launch(nc, maps):
    return run_bass_kernel_spmd(nc, maps, core_ids=list(range(8))).results


def _col(v):
    return np.ascontiguousarray(np.asarray(v, np.float32).reshape(16, 128).T)


def kernel(x, c, positions, ada_w, ada_b, norm_mix_g, norm_ffn_g,
           gdn_w_in, gdn_conv_w, gdn_a_log, gdn_dt_bias, gdn_o_gain, gdn_w_out,
           kv_norm_g, kv_ada_w, kv_ada_b, kv_w,
           diff_w_q, diff_lambda, diff_subln_g, diff_w_out,
           peer_w_q, peer_sub_keys, peer_u, peer_v, final_g):
    A = lambda a: np.asarray(a)
    x, c, positions = A(x), A(c), A(positions)
    B, S, _ = x.shape
    T = S // 4
    W = 2048
    f32 = np.float32
    ident = np.eye(128, dtype=f32)
    cT = np.ascontiguousarray(c.T.reshape(16, 128, 2).transpose(1, 0, 2)).astype(f32)
    maps = []
    for j in range(8):
        Wc = np.concatenate([A(ada_w[l])[:, j * 1536:(j + 1) * 1536] for l in range(4)] + [A(kv_ada_w)[:, j * 512:(j + 1) * 512]], axis=1)
        bc = np.concatenate([A(ada_b[l])[j * 1536:(j + 1) * 1536] for l in range(4)] + [A(kv_ada_b)[j * 512:(j + 1) * 512]])[None, :]
        maps.append(dict(cT=cT, W=np.ascontiguousarray(Wc), b=np.ascontiguousarray(bc)))
    res = _launch(_prog("ada", lambda: build_ada(6656)), maps)
    ada = np.zeros((4, 2, 12288), f32); kvada = np.zeros((2, 4096), f32)
    for j in range(8):
        o = res[j]["out"]
        for l in range(4):
            ada[l][:, j * 1536:(j + 1) * 1536] = o[:, l * 1536:(l + 1) * 1536]
        kvada[:, j * 512:(j + 1) * 512] = o[:, 4 * 1536:]
    xT = [np.ascontiguousarray(x[b].T) for b in range(B)]
    posb = [np.ascontiguousarray(np.broadcast_to(positions[b][None, :], (128, S))).astype(np.int32) for b in range(B)]
    ropec = rope_consts()
    kT = v_tok = None

    def run_proj(key, NR, NV, qscale, Wm, vecs_b):
        Wm = np.ascontiguousarray(A(Wm)); Wsw = swap_cols(Wm, NR)
        maps = []
        for b in range(B):
            for q in range(4):
                sl = slice(q * T, (q + 1) * T)
                maps.append(dict(xT=np.ascontiguousarray(xT[b][:, sl]), W=Wm, Wsw=Wsw, vecs=vecs_b[b],
                                 posb=np.ascontiguousarray(posb[b][:, sl]), invf=ropec))
        res = _launch(_prog(key, lambda: build_proj(T, NR, NV, qscale)), maps)
        return [np.concatenate([res[b * 4 + q]["outT"] for q in range(4)], axis=1) for b in range(B)]

    for l in range(4):
        sh1, sc1, gt1, sh2, sc2, gt2 = np.split(ada[l], 6, axis=-1)
        if l < 2:
            w_in, conv_w = A(gdn_w_in[l]), A(gdn_conv_w[l])
            cst = gdn_consts()
            maps = []
            for b in range(B):
                vecs = np.ascontiguousarray(np.stack([_col(sh1[b]), _col(sc1[b]), _col(A(norm_mix_g[l]))], axis=1))
                for hg in range(4):
                    heads = range(hg * 4, hg * 4 + 4)
                    cols = [w_in[:, kind * W + hh * 128: kind * W + (hh + 1) * 128] for hh in heads for kind in range(4)]
                    w_ab = np.concatenate([w_in[:, 4 * W + hg * 4: 4 * W + hg * 4 + 4], w_in[:, 4 * W + 16 + hg * 4: 4 * W + 16 + hg * 4 + 4]], axis=1)
                    cwl = [conv_w[:, kind * W + hh * 128: kind * W + (hh + 1) * 128].T for hh in heads for kind in range(3)]
                    hp = np.broadcast_to(np.stack([A(gdn_a_log[l])[hg * 4:hg * 4 + 4], A(gdn_dt_bias[l])[hg * 4:hg * 4 + 4]])[None], (128, 2, 4))
                    maps.append(dict(xT=xT[b], w_qkvz=np.ascontiguousarray(np.concatenate(cols, axis=1)), w_ab=np.ascontiguousarray(w_ab),
                                     convw=np.ascontiguousarray(np.stack(cwl, axis=1)), vecs=vecs, hp=np.ascontiguousarray(hp).astype(f32),
                                     ogain=np.ascontiguousarray(A(gdn_o_gain[l]).reshape(128, 1)), consts=cst))
            res = _launch(_prog("gdn", lambda: build_gdn(S)), maps)
            oT = [np.concatenate([res[b * 4 + hg]["ogT"] for hg in range(4)], axis=0) for b in range(B)]
            w_o = A(gdn_w_out[l])
        else:
            j = l - 2
            lambda_init = 0.8 - 0.6 * math.exp(-0.3 * l)
            vecs_b = [np.ascontiguousarray(np.stack([_col(sh1[b]), _col(sc1[b]), _col(A(norm_mix_g[l]))], axis=1)) for b in range(B)]
            qTf = run_proj("projq", 2048, 0, 128.0 ** -0.5, diff_w_q[j], vecs_b)
            lamp = np.ascontiguousarray(np.broadcast_to(A(diff_lambda[j])[None], (128, 4, 128))).astype(f32)
            gsub = np.ascontiguousarray(np.broadcast_to(A(diff_subln_g[j])[None], (128, 256))).astype(f32)
            ac = attn_consts()
            maps = []
            for b in range(B):
                q4 = qTf[b].reshape(8, 2, 128, S)
                for hp_ in range(4):
                    maps.append(dict(qT=np.ascontiguousarray(q4[2 * hp_:2 * hp_ + 2]), kT=np.ascontiguousarray(kT[b][2 * hp_:2 * hp_ + 2]),
                                     v=np.ascontiguousarray(v_tok[b][:, 2 * hp_:2 * hp_ + 2]), lamp=lamp, gsub=gsub, consts=ac))
            res = _launch(_prog(("attn", l), lambda: build_attn(S, lambda_init)), maps)
            oT = []
            for b in range(B):
                o = np.concatenate([res[b * 4 + hp_]["out"] for hp_ in range(4)], axis=1)
                oT.append(np.ascontiguousarray(o.reshape(S, 2048).T))
            w_o = A(diff_w_out[j])
        UTm = np.ascontiguousarray(A(peer_u[l]).T); Vm = np.ascontiguousarray(A(peer_v[l]))
        keysT = np.ascontiguousarray(A(peer_sub_keys[l]).transpose(2, 0, 1))
        w_o = np.ascontiguousarray(w_o); w_pq = np.ascontiguousarray(A(peer_w_q[l]))
        maps = []
        for b in range(B):
            vecs = np.ascontiguousarray(np.stack([_col(gt1[b]), _col(sh2[b]), _col(sc2[b]), _col(gt2[b]), _col(A(norm_ffn_g[l])), _col(A(final_g))], axis=1))
            for q in range(4):
                sl = slice(q * T, (q + 1) * T)
                maps.append(dict(xT=np.ascontiguousarray(xT[b][:, sl]), oT=np.ascontiguousarray(oT[b][:, sl]), w_out=w_o, w_q=w_pq, vecs=vecs,
                                 keysT=keysT, UT=UTm, Vt=Vm, ident=ident))
        fin = (l == 3)
        res = _launch(_prog(("mid", fin), lambda: build_mid(T, 128, final=fin)), maps)
        del UTm, Vm, maps
        xT = [np.concatenate([res[b * 4 + q]["x2T"] for q in range(4)], axis=1) for b in range(B)]
        if l == 1:
            ksh, ksc = np.split(kvada, 2, axis=-1)
            vecs_b = [np.ascontiguousarray(np.stack([_col(ksh[b]), _col(ksc[b]), _col(A(kv_norm_g))], axis=1)) for b in range(B)]
            kvT = run_proj("projkv", 2048, 2048, 1.0, kv_w, vecs_b)
            kT = [kvT[b][:2048].reshape(8, 2, 128, S) for b in range(B)]
            v_tok = [np.ascontiguousarray(kvT[b][2048:].T).reshape(S, 8, 256) for b in range(B)]
    return np.ascontiguousarray(np.stack([xT[b].T for b in range(B)], axis=0)).astype(f32)
```

```python
from contextlib import ExitStack
import math
import numpy as np
import concourse.bass as bass
import concourse.mybir as mybir
from concourse.bass_utils import run_bass_kernel_spmd

F32 = mybir.dt.float32
I32 = mybir.dt.int32
AF = mybir.ActivationFunctionType
ALU = mybir.AluOpType
AX = mybir.AxisListType
D = 2048
KC = 16
EPS = 1e-6
NH = 4


class StopBuild(Exception):
    pass


class Prog:
    ENG = ("pe", "act", "dve", "pool", "sp")

    def __init__(self):
        self.nc = bass.Bass("TRN2", target_bir_lowering=False, num_devices=8)
        self.gstack = ExitStack()
        self.stack = None
        self.ops = {e: [] for e in self.ENG}
        self.cnt = {}
        self.sems = {}
        self.waited = {e: {} for e in self.ENG}
        self.prefix = {e: [] for e in self.ENG}
        self.last_w = {}
        self.readers = {}
        self.total = 0
        self.gen = {e: 0 for e in self.ENG}
        self.own = {e: f"e_{e}_0" for e in self.ENG}
        self.pid = self.nc.partition_id()

    def sem(self, name):
        if name not in self.sems:
            self.sems[name] = self.gstack.enter_context(self.nc.semaphore(name))
            self.cnt[name] = 0
        return self.sems[name]

    def sbuf(self, name, shape, dtype=F32):
        return self.stack.enter_context(self.nc.sbuf_tensor(f"{name}__{self.sid}", list(shape), dtype))

    def psum(self, name, shape, dtype=F32):
        return self.stack.enter_context(self.nc.psum_tensor(f"{name}__{self.sid}", list(shape), dtype))

    def dram(self, name, shape, dtype=F32, kind="Internal"):
        return self.nc.dram_tensor(name, list(shape), dtype, kind=kind).ap()

    def begin(self):
        self.stack = ExitStack()
        self.sid = getattr(self, "sid", 0) + 1
        for e in self.ENG:
            if self.cnt.get(self.own[e], 0) > 24000:
                self.gen[e] += 1
                self.own[e] = f"e_{e}_{self.gen[e]}"

    def end(self, barrier=False):
        nc = self.nc
        sems, ops, prefix = self.sems, self.ops, self.prefix

        def emit(e, eng):
            for ws, wv in prefix[eng]:
                e.wait_ge(sems[ws], wv)
            for waits, fn, s, inc in ops[eng]:
                for ws, wv in waits:
                    e.wait_ge(sems[ws], wv)
                fn(e).then_inc(sems[s], inc)

        with nc.Block() as block:
            @block.tensor
            def _(e):
                emit(e, "pe")

            @block.scalar
            def _(e):
                emit(e, "act")

            @block.vector
            def _(e):
                emit(e, "dve")

            @block.gpsimd
            def _(e):
                emit(e, "pool")

            @block.sync
            def _(e):
                emit(e, "sp")
        self.total += sum(len(v) for v in ops.values())
        self.stack.close()
        self.stack = None
        for e in self.ENG:
            self.ops[e] = []
            pf = []
            for s, v in self.cnt.items():
                if v > 0 and self.waited[e].get(s, 0) < v:
                    pf.append((s, v))
                    self.waited[e][s] = v
            self.prefix[e] = pf
        if barrier:
            nc.all_core_barrier()

    def finish(self):
        self._finishing = True
        self.begin()
        self.end()
        self.gstack.close()
        return self.nc

    def _deps(self, reads, writes):
        d = {}

        def add(ev):
            if ev is None:
                return
            s, v = ev
            if d.get(s, 0) < v:
                d[s] = v
        for k in reads:
            add(self.last_w.get(k))
        for k in writes:
            add(self.last_w.get(k))
            for s, v in self.readers.get(k, {}).items():
                add((s, v))
        return d

    def _commit(self, ev, reads, writes):
        for k in writes:
            self.last_w[k] = ev
            self.readers[k] = {}
        for k in reads:
            r = self.readers.setdefault(k, {})
            if r.get(ev[0], 0) < ev[1]:
                r[ev[0]] = ev[1]

    def _waits(self, eng, deps, own):
        w = []
        for s, v in deps.items():
            if s == own and eng == "pe":
                continue
            if self.waited[eng].get(s, 0) >= v:
                continue
            self.waited[eng][s] = v
            w.append((s, v))
        return w

    @staticmethod
    def _keys(aps):
        return [a.name for a in aps if a is not None and hasattr(a, "name")]

    def op(self, eng, fn, reads, writes):
        own = self.own[eng]
        self.sem(own)
        rk, wk = self._keys(reads), self._keys(writes)
        deps = self._deps(rk, wk)
        waits = self._waits(eng, deps, own)
        self.cnt[own] += 1
        ev = (own, self.cnt[own])
        self.ops[eng].append((waits, fn, own, 1))
        self._commit(ev, rk, wk)

    def dma(self, out, in_, eng="sp", **kw):
        rk, wk = self._keys([in_]), self._keys([out])
        sname = "d_" + (wk[0] if wk else rk[0]).split("__")[0]
        self.sem(sname)
        deps = self._deps(rk, wk)
        if self.cnt[sname] > 0 and deps.get(sname, 0) < self.cnt[sname]:
            deps[sname] = self.cnt[sname]
        waits = self._waits(eng, deps, None)
        self.cnt[sname] += 16
        ev = (sname, self.cnt[sname])
        def _issue(e, out=out, in_=in_, kw=kw):
            try:
                return e.dma_start(out=out, in_=in_, **kw)
            except Exception:
                print("DMA FAILED", out.name, out.shape, in_.name, in_.shape, flush=True)
                raise
        self.ops[eng].append((waits, _issue, sname, 16))
        self._commit(ev, rk, wk)

    def allreduce(self, buf):
        flat = buf.flatten_outer_dims().rearrange("(o r) c -> o (r c)", o=1)
        self.op("pool", lambda e: e.collective_compute("AllReduce", ALU.add, replica_groups=[list(range(8))],
                                                        ins=[flat], outs=[flat]), [buf], [buf])

    def mm(self, out, lhsT, rhs, start=True, stop=True):
        self.op("pe", lambda e: e.matmul(out, lhsT, rhs, start=start, stop=stop), [lhsT, rhs], [out])

    def transpose(self, out, in_, ident):
        self.op("pe", lambda e: e.transpose(out, in_, ident), [in_, ident], [out])

    def act(self, out, in_, func, bias=None, scale=None):
        kw = {}
        if bias is not None:
            kw["bias"] = bias
        if scale is not None:
            kw["scale"] = scale
        self.op("act", lambda e: e.activation(out, in_, func, **kw), [in_, bias, scale], [out])

    def tt(self, out, in0, in1, op, eng="dve"):
        self.op(eng, lambda e: e.tensor_tensor(out, in0, in1, op), [in0, in1], [out])

    def ts(self, out, in0, s1, s2, op0, op1=None, eng="dve"):
        if op1 is None:
            self.op(eng, lambda e: e.tensor_scalar(out, in0, s1, None, op0), [in0, s1], [out])
        else:
            self.op(eng, lambda e: e.tensor_scalar(out, in0, s1, s2, op0, op1), [in0, s1, s2], [out])

    def stt(self, out, in0, scalar, in1, op0, op1):
        self.op("dve", lambda e: e.scalar_tensor_tensor(out, in0, scalar, in1, op0, op1), [in0, scalar, in1], [out])

    def copy(self, out, in_, eng="dve"):
        if eng == "act":
            self.op("act", lambda e: e.copy(out, in_), [in_], [out])
        else:
            self.op(eng, lambda e: e.tensor_copy(out, in_), [in_], [out])

    def memset(self, out, val, eng="dve"):
        self.op(eng, lambda e: e.memset(out, val), [], [out])

    def reduce(self, out, in_, op, axis=AX.X):
        self.op("dve", lambda e: e.tensor_reduce(out, in_, axis, op), [in_], [out])

    def vmax8(self, out, in_):
        self.op("dve", lambda e: e.max(out, in_), [in_], [out])

    def match_replace(self, out, to_replace, values, imm):
        self.op("dve", lambda e: e.match_replace(out, to_replace, values, imm), [to_replace, values], [out])

    def recip(self, out, in_):
        self.op("dve", lambda e: e.reciprocal(out, in_), [in_], [out])


def ds(start, size):
    return bass.ds(start, size)


def _view(XW, shape):
    names = "abcd"[:len(shape)]
    pat = "(" + " ".join(names) + ") -> " + " ".join(names)
    return XW.rearrange(pat, **{n: int(v) for n, v in zip(names, shape)})


def _merge01(ap):
    nd = len(ap.shape)
    names = "abcd"[:nd]
    return ap.rearrange(" ".join(names) + " -> (" + names[0] + " " + names[1] + ") " + " ".join(names[2:]))


def stage_exchange(P, XW, parts):
    P.begin()
    z = P.sbuf("zt", [128, 8192])
    P.memset(z[:], 0.0)
    n = XW.shape[0]
    f = min(8192, n // 128)
    v = XW.rearrange("(n p f) -> n p f", p=128, f=f)
    flat = XW.rearrange("(o n) -> o n", o=1)
    for shape, src_local, copies in parts:
        for i in range(v.shape[0]):
            P.dma(v[i], z[:, :f], eng="sp" if i % 2 == 0 else "pool")
        view = _view(XW, shape)
        P.dma(_merge01(view[ds(P.pid, 1)]), src_local, eng="pool")
        P.op("pool", lambda e: e.collective_compute("AllReduce", ALU.add, replica_groups=[list(range(8))],
                                                     ins=[flat], outs=[flat]), [XW], [XW])
        for dst, fn, eng in copies:
            P.dma(dst, fn(view), eng=eng)
    P.end()


def stage_ada(P, cT, W, bcol, adaX, adaloc, NCH):
    P.begin()
    c_sb = P.sbuf("c_sb", [128, 16, 2]); sc = P.sbuf("sc", [128, 16, 2])
    b_sb = P.sbuf("b_sb", [128, NCH]); o_sb = P.sbuf("o_sb", [128, 2, NCH])
    wb = [P.sbuf(f"wb{i}", [128, 16, 512]) for i in range(2)]
    ps = [P.psum(f"ps{i}", [128, 4, 2]) for i in range(2)]
    P.dma(c_sb[:], cT); P.dma(b_sb[:], bcol)
    P.act(sc[:], c_sb[:], AF.Silu)
    Wv = W.rearrange("(k p) n -> p k n", p=128)
    for t in range(NCH // 4):
        w = wb[t % 2]
        P.dma(w[:], Wv[:, :, t * 512:(t + 1) * 512])
        pp = ps[t % 2]
        for ci in range(4):
            for k in range(16):
                P.mm(pp[:, ci, :], w[:, k, ci * 128:(ci + 1) * 128], sc[:, k, :], start=(k == 0), stop=(k == 15))
        for bb in range(2):
            P.tt(o_sb[:, bb, t * 4:(t + 1) * 4], pp[:, :, bb], b_sb[:, t * 4:(t + 1) * 4], ALU.add)
    P.dma(adaX, o_sb[:])
    P.end()


def load_vec(P, dst, adaX, bidx, l, v, per_core=12, base=0, lstride=12):
    k = 0
    while k < 16:
        g = v * 16 + k
        j, ci = g // per_core, g % per_core
        n = min(16 - k, per_core - ci)
        c0 = base + l * lstride + ci
        P.dma(dst[:, k:k + n], adaX[j, :, c0:c0 + n])
        k += n


def rms_rstd(P, xs, sq, ps, ones, rstd, n, width):
    for j in range(KC):
        P.act(sq[j % 2][:, :width], xs[:, j, :], AF.Square)
        P.mm(ps[:, :width], ones, sq[j % 2][:, :width], start=(j == 0), stop=(j == KC - 1))
    P.act(rstd[:, :width], ps[:, :width], AF.Sqrt, bias=EPS, scale=1.0 / n)
    P.recip(rstd[:, :width], rstd[:, :width])


def stage_gdn(P, S, T, xX, adaX, l, w, oX, consts):
    P.begin()
    bq = (P.pid // 4) * 4
    bidx = P.pid // 4
    TT = 512
    NSUB = TT // 128
    sh1 = P.sbuf("sh1", [128, KC]); A1 = P.sbuf("A1", [128, KC]); gm = P.sbuf("gm", [128, KC])
    hps = P.sbuf("hps", [128, 2, NH]); negA = P.sbuf("negA", [128, NH])
    og = P.sbuf("og", [128, 1]); cw = P.sbuf("cw", [128, 12, 4])
    cst = P.sbuf("cst", [128, 4, 128])
    ident, triI, triS, ones = cst[:, 0, :], cst[:, 1, :], cst[:, 2, :], cst[:, 3, :]
    wabs = P.sbuf("wabs", [128, KC, 8])
    xs = P.sbuf("xs", [128, KC, TT]); hs = P.sbuf("hs", [128, KC, TT])
    wblk = [P.sbuf(f"wblk{i}", [128, KC, 128]) for i in range(2)]
    sq = [P.sbuf(f"sq{i}", [128, TT]) for i in range(2)]
    rstd = P.sbuf("rstd", [128, TT])
    pr = [P.sbuf(f"pr{c}", [128, 3 + TT]) for c in range(16)]
    cv = [P.sbuf(f"cv{c}", [128, TT]) for c in range(12)]
    oT = [P.sbuf(f"oT{h}", [128, TT]) for h in range(NH)]
    ab = [P.sbuf(f"ab{s}", [128, 8]) for s in range(NSUB)]
    Sst = [P.sbuf(f"Sst{h}", [128, 128]) for h in range(NH)]

    def hb(name):
        return [P.sbuf(f"{name}{h}", [128, 128]) for h in range(NH)]
    gcol = P.sbuf("gcol", [128, NH]); beta = P.sbuf("beta", [128, NH]); nbeta = P.sbuf("nbeta", [128, NH])
    l4 = P.sbuf("l4", [128, NH]); p4 = P.sbuf("p4", [128, NH]); m4 = P.sbuf("m4", [128, NH])
    u4 = P.sbuf("u4", [128, NH]); Gc = P.sbuf("Gc", [128, NH]); eG = P.sbuf("eG", [128, NH]); neG = P.sbuf("neG", [128, NH])
    dcol = P.sbuf("dcol", [128, NH]); gl = P.sbuf("gl", [128, NH]); Glast = P.sbuf("Glast", [128, NH])
    gb = hb("gb"); arg = hb("arg"); E = hb("E"); DTi = hb("DTi"); DTs = hb("DTs"); EGb = hb("EGb")
    AqkT = hb("AqkT"); Pm = [hb("Pm0"), hb("Pm1")]; Qm = [hb("Qm0"), hb("Qm1")]; X = hb("X")
    Ktok = hb("Ktok"); Vtok = hb("Vtok"); Rp = hb("Rp"); un = hb("un"); QgT = hb("QgT")
    psBig = [P.psum(f"psBig{i}", [128, 512]) for i in range(2)]
    psHb = [P.psum(f"psHb{i}", [128, 256]) for i in range(6)]
    psH = [[psHb[(h * 3 + i) // 2][:, ((h * 3 + i) % 2) * 128:((h * 3 + i) % 2) * 128 + 128] for i in range(3)] for h in range(NH)]

    xb = xX; ogl = oX
    load_vec(P, sh1, adaX, bidx, l, 0); load_vec(P, A1, adaX, bidx, l, 1)
    P.dma(gm[:], w["g_mix"]); P.dma(hps[:], w["hp"]); P.dma(og[:], w["ogain"]); P.dma(cw[:], w["convw"]); P.dma(cst[:], consts)
    P.dma(wabs[:], w["w_ab"].rearrange("(k p) n -> p k n", p=128))
    P.ts(A1[:], A1[:], 1.0, None, ALU.add)
    P.tt(A1[:], A1[:], gm[:], ALU.mult)
    P.act(negA[:], hps[:, 0, :], AF.Exp)
    P.ts(negA[:], negA[:], -1.0, None, ALU.mult)
    for h in range(NH):
        P.memset(Sst[h][:], 0.0)
    for c in range(16):
        P.memset(pr[c][:, 0:3], 0.0)
    wqv = w["w_qkvz"].rearrange("(k p) n -> p k n", p=128)
    wi = 0
    for tl in range(S // TT):
        t0 = tl * TT
        blk, off = t0 // T, t0 % T
        P.dma(xs[:], xb[blk].rearrange("(k p) t -> p k t", p=128)[:, :, off:off + TT])
        rms_rstd(P, xs, sq, psBig[0], ones, rstd, D, TT)
        for j in range(KC):
            P.stt(sq[j % 2][:], xs[:, j, :], A1[:, j:j + 1], rstd[:], ALU.mult, ALU.mult)
            P.act(hs[:, j, :], sq[j % 2][:], AF.Identity, bias=sh1[:, j:j + 1])
        for c in range(16):
            h, kind = c // 4, c % 4
            wk = wblk[wi % 2]; wi += 1
            P.dma(wk[:], wqv[:, :, c * 128:(c + 1) * 128])
            pb = psBig[c % 2]
            for k in range(KC):
                P.mm(pb[:], wk[:, k, :], hs[:, k, :], start=(k == 0), stop=(k == KC - 1))
            if kind < 3 and tl > 0:
                P.copy(pr[c][:, 0:3], pr[c][:, TT:TT + 3], eng="pool")
            P.copy(pr[c][:, 3:3 + TT], pb[:], eng="act")
        for s in range(NSUB):
            pb = psBig[s % 2]
            for k in range(KC):
                P.mm(pb[:, 0:8], hs[:, k, s * 128:(s + 1) * 128], wabs[:, k, :], start=(k == 0), stop=(k == KC - 1))
            P.copy(ab[s][:], pb[:, 0:8])
        for h in range(NH):
            for kind in range(3):
                c = h * 4 + kind; ci = h * 3 + kind
                o = cv[ci]
                P.ts(o[:], pr[c][:, 0:TT], cw[:, ci, 0:1], None, ALU.mult)
                for i in range(1, 4):
                    P.stt(o[:], pr[c][:, i:i + TT], cw[:, ci, i:i + 1], o[:], ALU.mult, ALU.add)
                P.act(o[:], o[:], AF.Silu)
                if kind < 2:
                    P.act(sq[kind][:], o[:], AF.Square)
                    pb = psBig[kind]
                    P.mm(pb[:], ones, sq[kind][:])
                    P.act(sq[kind][:], pb[:], AF.Sqrt, bias=EPS)
                    P.recip(sq[kind][:], sq[kind][:])
                    if kind == 0:
                        P.stt(o[:], o[:], 128.0 ** -0.5, sq[kind][:], ALU.mult, ALU.mult)
                    else:
                        P.tt(o[:], o[:], sq[kind][:], ALU.mult)
        for s in range(NSUB):
            cs = slice(s * 128, (s + 1) * 128)
            P.tt(u4[:], ab[s][:, 0:NH], hps[:, 1, :], ALU.add)
            P.act(u4[:], u4[:], AF.Exp)
            P.act(l4[:], u4[:], AF.Ln, bias=1.0)
            P.ts(p4[:], u4[:], -0.2, 0.25, ALU.mult, ALU.add)
            for cst_ in (1.0 / 3, 0.5, 1.0):
                P.tt(p4[:], p4[:], u4[:], ALU.mult)
                P.ts(p4[:], p4[:], -1.0, cst_, ALU.mult, ALU.add)
            P.tt(p4[:], p4[:], u4[:], ALU.mult)
            P.ts(m4[:], u4[:], 0.1, None, ALU.is_lt)
            P.tt(p4[:], p4[:], l4[:], ALU.subtract)
            P.tt(p4[:], p4[:], m4[:], ALU.mult)
            P.tt(u4[:], l4[:], p4[:], ALU.add)
            P.tt(gcol[:], u4[:], negA[:], ALU.mult)
            P.act(beta[:], ab[s][:, NH:2 * NH], AF.Sigmoid)
            P.ts(nbeta[:], beta[:], -1.0, None, ALU.mult)
            P.mm(psBig[0][:, 0:NH], triI, gcol[:])
            P.copy(Gc[:], psBig[0][:, 0:NH])
            P.act(eG[:], Gc[:], AF.Exp)
            P.ts(neG[:], eG[:], -1.0, None, ALU.mult)
            for h in range(NH):
                qT, kT, vT = cv[h * 3][:, cs], cv[h * 3 + 1][:, cs], cv[h * 3 + 2][:, cs]
                p0, p1, p2 = psH[h]
                P.ts(gb[h][:], ones, gcol[:, h:h + 1], None, ALU.mult)
                P.mm(p0, gb[h][:], triI)
                P.copy(Glast[:, h:h + 1], p0[:, 127:128])
                P.ts(arg[h][:], p0, Gc[:, h:h + 1], 0.0, ALU.subtract, ALU.min)
                P.act(E[h][:], arg[h][:], AF.Exp)
                P.act(EGb[h][:], p0, AF.Exp)
                P.tt(DTi[h][:], E[h][:], triI, ALU.mult)
                P.tt(DTs[h][:], E[h][:], triS, ALU.mult)
                P.act(dcol[:, h:h + 1], Gc[:, h:h + 1], AF.Exp, scale=-1.0, bias=Glast[:, h:h + 1])
                P.act(gl[:, h:h + 1], Glast[:, h:h + 1], AF.Exp)
                P.tt(QgT[h][:], qT, EGb[h][:], ALU.mult)
                P.mm(p1, kT, kT)
                M = Pm[0][h]
                P.stt(M[:], p1, nbeta[:, h:h + 1], DTs[h][:], ALU.mult, ALU.mult)
                P.mm(p2, kT, qT)
                P.tt(AqkT[h][:], p2, DTi[h][:], ALU.mult)
                P.transpose(p1, M[:], ident)
                P.copy(Qm[0][h][:], p1, eng="act")
                P.tt(X[h][:], M[:], ident, ALU.add)
                P.transpose(p2, kT, ident)
                P.ts(Ktok[h][:], p2, dcol[:, h:h + 1], None, ALU.mult)
                P.transpose(p0, vT, ident)
                P.copy(Vtok[h][:], p0, eng="act")
            for lv in range(6):
                a, b_ = lv % 2, (lv + 1) % 2
                for h in range(NH):
                    p0, p1, p2 = psH[h]
                    P.mm(p1, Pm[a][h][:], Qm[a][h][:])
                    P.copy(Qm[b_][h][:], p1, eng="act")
                    if lv < 5:
                        P.mm(p0, Qm[a][h][:], Pm[a][h][:])
                        P.copy(Pm[b_][h][:], p0)
                    P.mm(p2, Qm[b_][h][:], X[h][:])
                    P.tt(X[h][:], X[h][:], p2, ALU.add)
            for h in range(NH):
                kT = cv[h * 3 + 1][:, cs]
                p0, p1, p2 = psH[h]
                P.mm(p0, kT, Sst[h][:])
                P.stt(Rp[h][:], p0, neG[:, h:h + 1], Vtok[h][:], ALU.mult, ALU.add)
            for h in range(NH):
                p0, p1, p2 = psH[h]
                P.mm(p1, X[h][:], Rp[h][:])
                P.ts(un[h][:], p1, beta[:, h:h + 1], None, ALU.mult)
            for h in range(NH):
                p0, p1, p2 = psH[h]
                P.mm(p2, Sst[h][:], QgT[h][:], start=True, stop=False)
                P.mm(p2, un[h][:], AqkT[h][:], start=False, stop=True)
                P.copy(oT[h][:, cs], p2, eng="act")
                P.mm(p0, Ktok[h][:], un[h][:])
                P.stt(Sst[h][:], Sst[h][:], gl[:, h:h + 1], p0, ALU.mult, ALU.add)
        for h in range(NH):
            zc = pr[h * 4 + 3]
            P.act(sq[0][:], oT[h][:], AF.Square)
            pb = psBig[h % 2]
            P.mm(pb[:], ones, sq[0][:])
            P.act(sq[1][:], pb[:], AF.Sqrt, bias=EPS, scale=1.0 / 128)
            P.recip(sq[1][:], sq[1][:])
            P.stt(oT[h][:], oT[h][:], og[:, 0:1], sq[1][:], ALU.mult, ALU.mult)
            P.act(sq[0][:], zc[:, 3:3 + TT], AF.Silu)
            P.tt(oT[h][:], oT[h][:], sq[0][:], ALU.mult)
            P.dma(ogl[h * 128:(h + 1) * 128, t0:t0 + TT], oT[h][:])
    P.end()


def stage_mid(P, S, T, NK, xsrc, oX, adaX, l, w, xdst, xXnext, final, ident_d):
    P.begin()
    NE = NK * NK
    bq = (P.pid // 4) * 4
    bidx = P.pid // 4
    q = P.pid % 4
    ST = 256
    NSUB = ST // 128
    EI = min(16, NK); GW = EI * NK; NG = NE // GW; EB = 256; NB = GW // EB
    gt1 = P.sbuf("gt1", [128, KC]); sh2 = P.sbuf("sh2", [128, KC]); A2 = P.sbuf("A2", [128, KC]); gt2 = P.sbuf("gt2", [128, KC])
    gf = P.sbuf("gf", [128, KC]); fgn = P.sbuf("fgn", [128, KC])
    keys = P.sbuf("keys", [128, 2, NK])
    ident = P.sbuf("identsb", [128, 128]); ones = P.sbuf("ones", [128, 128])
    xs = P.sbuf("xs", [128, KC, ST]); os_ = P.sbuf("os", [128, KC, ST])
    wblk = [P.sbuf(f"wblk{i}", [128, KC, 128]) for i in range(2)]
    Ssc = [P.sbuf(f"S{i}", [128, 8, 2, NK]) for i in range(NSUB)]
    qTc = [P.sbuf(f"qTc{i}", [128, ST]) for i in range(2)]
    sq = [P.sbuf(f"sq{i}", [128, ST]) for i in range(2)]
    rstd = P.sbuf("rstd", [128, ST])
    t1 = P.sbuf("t1", [128, 16]); t2 = P.sbuf("t2", [128, 16]); bb = P.sbuf("bb", [128, 16]); eb = P.sbuf("eb", [128, 16])
    tmpk = P.sbuf("tmpk", [128, NK]); cand = P.sbuf("cand", [128, 16, 16]); tmpc = P.sbuf("tmpc", [128, 256])
    tau = [P.sbuf(f"tau{i}", [128, 8]) for i in range(NSUB)]
    negm = [P.sbuf(f"negm{i}", [128, 8]) for i in range(NSUB)]
    Z = [P.sbuf(f"Z{i}", [128, 8]) for i in range(NSUB)]
    invZ = [P.sbuf(f"invZ{i}", [128, 8]) for i in range(NSUB)]
    cbuf = P.sbuf("cbuf", [128, EI, NK]); ebuf = P.sbuf("ebuf", [128, EI, NK]); gbuf = P.sbuf("gbuf", [128, EI, NK])
    gate = P.sbuf("gate", [128, GW])
    UTb = [P.sbuf(f"UTb{i}", [128, KC, EB]) for i in range(2)]
    Vb = [P.sbuf(f"Vb{i}", [128, EB // 128, D]) for i in range(2)]
    ga = [P.sbuf(f"ga{i}", [128, EB]) for i in range(2)]
    gaT = [P.sbuf(f"gaT{i}", [128, EB // 128, 128]) for i in range(2)]
    acc = P.sbuf("acc", [128, D])
    psA = [P.psum(f"psA{i}", [128, 512]) for i in range(2)]
    psB = [P.psum(f"psB{i}", [128, 512]) for i in range(2)]
    psO = P.psum("psO", [128, D])

    ol = oX
    load_vec(P, gt1, adaX, bidx, l, 2); load_vec(P, sh2, adaX, bidx, l, 3); load_vec(P, A2, adaX, bidx, l, 4); load_vec(P, gt2, adaX, bidx, l, 5)
    P.dma(gf[:], w["g_ffn"]); P.dma(fgn[:], w["final_g"])
    P.dma(keys[:], w["keysT"]); P.dma(ident[:], ident_d)
    P.memset(ones[:], 1.0)
    P.ts(A2[:], A2[:], 1.0, None, ALU.add)
    P.tt(A2[:], A2[:], gf[:], ALU.mult)
    wov = w["w_out"].rearrange("(k p) n -> p k n", p=128)
    wqv = w["w_pq"].rearrange("(k p) n -> p k n", p=128)
    UTv = w["UT"].rearrange("(k p) e -> p k e", p=128)
    Vt = w["Vt"]
    wi = 0
    for st in range(T // ST):
        t0 = st * ST
        P.dma(xs[:], xsrc.rearrange("(k p) t -> p k t", p=128)[:, :, t0:t0 + ST])
        for hg in range(4):
            P.dma(os_[:, hg * 4:(hg + 1) * 4, :], ol[hg].rearrange("(k p) t -> p k t", p=128)[:, :, t0:t0 + ST])
        for j in range(KC):
            wk = wblk[wi % 2]; wi += 1
            P.dma(wk[:], wov[:, :, j * 128:(j + 1) * 128])
            pa = psA[j % 2]
            for k in range(KC):
                P.mm(pa[:, :ST], wk[:, k, :], os_[:, k, :], start=(k == 0), stop=(k == KC - 1))
            P.stt(xs[:, j, :], pa[:, :ST], gt1[:, j:j + 1], xs[:, j, :], ALU.mult, ALU.add)
        rms_rstd(P, xs, sq, psB[0], ones[:], rstd, D, ST)
        for j in range(KC):
            P.stt(sq[j % 2][:], xs[:, j, :], A2[:, j:j + 1], rstd[:], ALU.mult, ALU.mult)
            P.act(os_[:, j, :], sq[j % 2][:], AF.Identity, bias=sh2[:, j:j + 1])
        for c in range(16):
            p, jj = c // 2, c % 2
            wk = wblk[wi % 2]; wi += 1
            P.dma(wk[:], wqv[:, :, c * 128:(c + 1) * 128])
            pa = psA[c % 2]
            for k in range(KC):
                P.mm(pa[:, :ST], wk[:, k, :], os_[:, k, :], start=(k == 0), stop=(k == KC - 1))
            qq = qTc[c % 2]
            P.copy(qq[:], pa[:, :ST], eng="act")
            for sub in range(NSUB):
                pb = psB[sub % 2]
                P.mm(pb[:, :NK], qq[:, sub * 128:(sub + 1) * 128], keys[:, jj, :])
                P.copy(Ssc[sub][:, p, jj, :], pb[:, :NK])
        for sub in range(NSUB):
            for p in range(8):
                for (tk, jj) in ((t1, 0), (t2, 1)):
                    s_ = Ssc[sub][:, p, jj, :]
                    P.vmax8(tk[:, 0:8], s_)
                    P.match_replace(tmpk[:], tk[:, 0:8], s_, -1e30)
                    P.vmax8(tk[:, 8:16], tmpk[:])
                P.tt(cand[:], t1[:].unsqueeze(2).to_broadcast([128, 16, 16]),
                     t2[:].unsqueeze(1).to_broadcast([128, 16, 16]), ALU.add)
                cf = cand[:].rearrange("p a b -> p (a b)")
                P.vmax8(bb[:, 0:8], cf)
                P.match_replace(tmpc[:], bb[:, 0:8], cf, -1e30)
                P.vmax8(bb[:, 8:16], tmpc[:])
                P.copy(tau[sub][:, p:p + 1], bb[:, 15:16])
                P.ts(negm[sub][:, p:p + 1], bb[:, 0:1], -1.0, None, ALU.mult)
                P.act(eb[:], bb[:], AF.Exp, bias=negm[sub][:, p:p + 1])
                P.reduce(Z[sub][:, p:p + 1], eb[:], ALU.add)
            P.recip(invZ[sub][:], Z[sub][:])
        bi = 0
        for sub in range(NSUB):
            tsl = slice(sub * 128, (sub + 1) * 128)
            for g in range(NG):
                i0 = g * EI
                for p in range(8):
                    P.tt(cbuf[:], Ssc[sub][:, p, 0, i0:i0 + EI].unsqueeze(2).to_broadcast([128, EI, NK]),
                         Ssc[sub][:, p, 1, :].unsqueeze(1).to_broadcast([128, EI, NK]), ALU.add, eng="pool")
                    P.act(ebuf[:], cbuf[:], AF.Exp, bias=negm[sub][:, p:p + 1])
                    P.stt(gbuf[:], cbuf[:], tau[sub][:, p:p + 1], ebuf[:], ALU.is_ge, ALU.mult)
                    gfl = gbuf[:].rearrange("p a b -> p (a b)")
                    if p == 0:
                        P.ts(gate[:], gfl, invZ[sub][:, p:p + 1], None, ALU.mult)
                    else:
                        P.stt(gate[:], gfl, invZ[sub][:, p:p + 1], gate[:], ALU.mult, ALU.add)
                for b in range(NB):
                    e0 = g * GW + b * EB
                    ub = UTb[bi % 2]; vb = Vb[bi % 2]; gg = ga[bi % 2]; gt = gaT[bi % 2]
                    pa = psA[bi % 2]; pt = psB[bi % 2]
                    first = (g == 0 and b == 0); last = (g == NG - 1 and b == NB - 1)
                    bi += 1
                    P.dma(ub[:], UTv[:, :, e0:e0 + EB])
                    P.dma(vb[:], Vt[e0:e0 + EB, :].rearrange("(c p) d -> p c d", p=128), eng="sp")
                    for k in range(KC):
                        P.mm(pa[:, :EB], os_[:, k, tsl], ub[:, k, :], start=(k == 0), stop=(k == KC - 1))
                    P.act(gg[:], pa[:, :EB], AF.Gelu)
                    P.tt(gg[:], gg[:], gate[:, b * EB:(b + 1) * EB], ALU.mult)
                    for c in range(EB // 128):
                        P.transpose(pt[:, c * 128:(c + 1) * 128], gg[:, c * 128:(c + 1) * 128], ident[:])
                    P.copy(gt[:].rearrange("p c t -> p (c t)"), pt[:, :EB])
                    for dt in range(4):
                        for c in range(EB // 128):
                            P.mm(psO[:, dt * 512:(dt + 1) * 512], gt[:, c, :], vb[:, c, dt * 512:(dt + 1) * 512],
                                 start=(first and c == 0), stop=(last and c == EB // 128 - 1))
            P.copy(acc[:], psO[:], eng="act")
            for j in range(KC):
                pa = psA[j % 2]
                P.transpose(pa[:, :128], acc[:, j * 128:(j + 1) * 128], ident[:])
                P.stt(xs[:, j, tsl], pa[:, :128], gt2[:, j:j + 1], xs[:, j, tsl], ALU.mult, ALU.add)
        if final:
            rms_rstd(P, xs, sq, psB[0], ones[:], rstd, D, ST)
            for j in range(KC):
                P.stt(xs[:, j, :], xs[:, j, :], fgn[:, j:j + 1], rstd[:], ALU.mult, ALU.mult)
        P.dma(xdst.rearrange("(k p) t -> p k t", p=128)[:, :, t0:t0 + ST], xs[:])
    P.end()


def stage_proj(P, T, xsrc, adaX, vsrc, g_ap, W, Wsw, NR, NV, qscale, posb, invf, outX, vX, tag):
    P.begin()
    bidx = P.pid // 4
    TT = min(512, T)
    sh = P.sbuf("sh", [128, KC]); A1 = P.sbuf("A1", [128, KC]); gm = P.sbuf("gm", [128, KC]); ones = P.sbuf("ones", [128, 128])
    ivf = P.sbuf("ivf", [128, 2]); pi_ = P.sbuf("pi_", [128, TT], I32); ang = P.sbuf("ang", [128, TT])
    ki = P.sbuf("ki", [128, TT], I32); fx = P.sbuf("fx", [128, TT]); cosF = P.sbuf("cosF", [128, TT]); sinS = P.sbuf("sinS", [128, TT]); tmp = P.sbuf("tmp", [128, TT])
    xs = P.sbuf("xs", [128, KC, TT]); hs = P.sbuf("hs", [128, KC, TT])
    sq = [P.sbuf(f"sq{i}", [128, TT]) for i in range(2)]
    rstd = P.sbuf("rstd", [128, TT])
    wblk = [P.sbuf(f"wblk{i}", [128, KC, 128]) for i in range(2)]
    wsblk = [P.sbuf(f"wsblk{i}", [128, KC, 128]) for i in range(2)]
    ob = [P.sbuf(f"ob{i}", [128, TT]) for i in range(2)]
    ps = [P.psum(f"ps{i}", [128, 512]) for i in range(2)]
    ps2 = [P.psum(f"ps2{i}", [128, 512]) for i in range(2)]
    if NV:
        wv = P.sbuf("wv", [128, KC, 512]); vo = [P.sbuf(f"vo{i}", [128, 512]) for i in range(2)]
    outl = outX; vl = vX
    l, v_sh, v_sc, per_core, base, lstride = vsrc
    load_vec(P, sh, adaX, bidx, l, v_sh, per_core, base, lstride); load_vec(P, A1, adaX, bidx, l, v_sc, per_core, base, lstride)
    P.dma(gm[:], g_ap); P.dma(ivf[:], invf); P.memset(ones[:], 1.0)
    P.ts(A1[:], A1[:], 1.0, None, ALU.add)
    P.tt(A1[:], A1[:], gm[:], ALU.mult)
    Wv = W.rearrange("(k p) n -> p k n", p=128)
    Wsv = Wsw.rearrange("(k p) n -> p k n", p=128)
    TWO_PI = 2 * math.pi

    def sin_of(dst, src_ang):
        P.ts(tmp[:], src_ang, 1.0 / TWO_PI, None, ALU.mult)
        P.copy(ki[:], tmp[:])
        P.copy(tmp[:], ki[:])
        P.stt(tmp[:], tmp[:], -TWO_PI, src_ang, ALU.mult, ALU.add)
        P.ts(fx[:], tmp[:], math.pi, TWO_PI, ALU.is_gt, ALU.mult)
        P.tt(tmp[:], tmp[:], fx[:], ALU.subtract)
        P.ts(fx[:], tmp[:], -math.pi, TWO_PI, ALU.is_lt, ALU.mult)
        P.tt(tmp[:], tmp[:], fx[:], ALU.add)
        P.act(dst, tmp[:], AF.Sin)
    for tl in range(T // TT):
        t0 = tl * TT
        P.dma(xs[:], xsrc.rearrange("(k p) t -> p k t", p=128)[:, :, t0:t0 + TT])
        P.dma(pi_[:], posb[:, t0:t0 + TT])
        P.copy(ang[:], pi_[:])
        P.ts(ang[:], ang[:], ivf[:, 0:1], None, ALU.mult)
        sin_of(sinS[:], ang[:])
        P.ts(sinS[:], sinS[:], ivf[:, 1:2], None, ALU.mult)
        P.ts(sinS[:], sinS[:], qscale, None, ALU.mult)
        P.ts(ang[:], ang[:], math.pi / 2, None, ALU.add)
        sin_of(cosF[:], ang[:])
        P.ts(cosF[:], cosF[:], qscale, None, ALU.mult)
        rms_rstd(P, xs, sq, ps[0], ones[:], rstd, D, TT)
        for j in range(KC):
            P.stt(sq[j % 2][:], xs[:, j, :], A1[:, j:j + 1], rstd[:], ALU.mult, ALU.mult)
            P.act(hs[:, j, :], sq[j % 2][:], AF.Identity, bias=sh[:, j:j + 1])
        for c in range(NR // 128):
            wk = wblk[c % 2]; ws = wsblk[c % 2]
            P.dma(wk[:], Wv[:, :, c * 128:(c + 1) * 128])
            P.dma(ws[:], Wsv[:, :, c * 128:(c + 1) * 128], eng="pool")
            pa = ps[c % 2]; pb = ps2[c % 2]
            for k in range(KC):
                P.mm(pa[:, :TT], wk[:, k, :], hs[:, k, :], start=(k == 0), stop=(k == KC - 1))
            for k in range(KC):
                P.mm(pb[:, :TT], ws[:, k, :], hs[:, k, :], start=(k == 0), stop=(k == KC - 1))
            o = ob[c % 2]
            P.tt(o[:], pa[:, :TT], cosF[:], ALU.mult)
            P.tt(tmp[:], pb[:, :TT], sinS[:], ALU.mult)
            P.tt(o[:], o[:], tmp[:], ALU.add)
            P.dma(outl[c * 128:(c + 1) * 128, t0:t0 + TT], o[:])
        for cb in range(NV // 512):
            P.dma(wv[:], Wv[:, :, NR + cb * 512:NR + (cb + 1) * 512])
            for sub in range(TT // 128):
                pa = ps[sub % 2]
                for k in range(KC):
                    P.mm(pa[:], hs[:, k, sub * 128:(sub + 1) * 128], wv[:, k, :], start=(k == 0), stop=(k == KC - 1))
                o = vo[sub % 2]
                P.copy(o[:], pa[:], eng="act")
                P.dma(vl[cb, t0 + sub * 128:t0 + (sub + 1) * 128, :], o[:])
    P.end()


def stage_attn(P, S, T, qX, kl, vl, lamp, gsub, consts, lambda_init, oX, tag):
    P.begin()
    NHD = 2
    NB = S // 128
    bq = (P.pid // 4) * 4
    hp = P.pid % 4
    cst = P.sbuf("cst", [128, 2, 128]); ident, cmask = cst[:, 0, :], cst[:, 1, :]
    lp = P.sbuf("lp", [128, 4, 128]); lt = P.sbuf("lt", [128, 2, 128]); l2 = P.sbuf("l2", [128, 2]); neglam = P.sbuf("neglam", [128, 1])
    gs = P.sbuf("gs", [128, 256])
    ks = [P.sbuf(f"ks{c}", [128, S]) for c in range(2)]
    vs = P.sbuf("vs", [128, NB, 256])
    Pb = [P.sbuf(f"Pb{c}", [128, S]) for c in range(2)]
    qs = [[P.sbuf(f"qs{c}_{i}", [128, 128]) for i in range(2)] for c in range(2)]
    PT = [P.sbuf(f"PT{i}", [128, 128]) for i in range(2)]
    m = P.sbuf("m", [128, 2]); negm = P.sbuf("negm", [128, 2]); Z = P.sbuf("Z", [128, 2]); iZ = P.sbuf("iZ", [128, 2]); nl1 = P.sbuf("nl1", [128, 1])
    ob = [P.sbuf(f"ob{i}", [128, 256]) for i in range(2)]; osq = P.sbuf("osq", [128, 256]); ss = P.sbuf("ss", [128, 1])
    oTs = [P.sbuf(f"oTs{i}", [128, 2, 128]) for i in range(2)]
    psS = [P.psum(f"psS{i}", [128, 512]) for i in range(2)]
    psT = [P.psum(f"psT{i}", [128, 128]) for i in range(2)]
    psO = [P.psum(f"psO{i}", [128, 256]) for i in range(2)]
    ql = qX; oal = oX
    P.dma(cst[:], consts); P.dma(lp[:], lamp); P.dma(gs[:], gsub)
    P.tt(lt[:, 0, :], lp[:, 0, :], lp[:, 1, :], ALU.mult)
    P.tt(lt[:, 1, :], lp[:, 2, :], lp[:, 3, :], ALU.mult)
    P.reduce(l2[:], lt[:], ALU.add)
    P.act(l2[:], l2[:], AF.Exp)
    P.tt(neglam[:], l2[:, 1:2], l2[:, 0:1], ALU.subtract)
    P.ts(neglam[:], neglam[:], -lambda_init, None, ALU.add)
    NQ = S // T
    it = 0
    for hd in range(NHD):
        for c in range(2):
            for qq in range(NQ):
                P.dma(ks[c][:, qq * T:(qq + 1) * T], kl[qq, hd * 256 + c * 128:hd * 256 + (c + 1) * 128, :])
        for qq in range(NQ):
            P.dma(vs[:, qq * (T // 128):(qq + 1) * (T // 128), :],
                  vl[qq, :, hd * 256:(hd + 1) * 256].rearrange("(n p) d -> p n d", p=128))
        for qb in range(NB):
            nk = (qb + 1) * 128
            par = it % 2; it += 1
            qq, qoff = (qb * 128) // T, (qb * 128) % T
            for c in range(2):
                q = qs[c][par]
                P.dma(q[:], ql[qq, hd * 256 + c * 128:hd * 256 + (c + 1) * 128, qoff:qoff + 128])
                for kt in range((nk + 511) // 512):
                    wd = min(512, nk - kt * 512)
                    pp = psS[(kt + c) % 2]
                    P.mm(pp[:, :wd], q[:], ks[c][:, kt * 512:kt * 512 + wd])
                    P.copy(Pb[c][:, kt * 512:kt * 512 + wd], pp[:, :wd], eng="act" if kt % 2 else "dve")
                P.tt(Pb[c][:, nk - 128:nk], Pb[c][:, nk - 128:nk], cmask, ALU.add)
                P.reduce(m[:, c:c + 1], Pb[c][:, :nk], ALU.max)
                P.ts(negm[:, c:c + 1], m[:, c:c + 1], -1.0, None, ALU.mult)
                P.act(Pb[c][:, :nk], Pb[c][:, :nk], AF.Exp, bias=negm[:, c:c + 1])
                P.reduce(Z[:, c:c + 1], Pb[c][:, :nk], ALU.add)
            P.recip(iZ[:], Z[:])
            P.tt(nl1[:], iZ[:, 1:2], neglam[:], ALU.mult)
            P.ts(Pb[0][:, :nk], Pb[0][:, :nk], iZ[:, 0:1], None, ALU.mult)
            P.stt(Pb[0][:, :nk], Pb[1][:, :nk], nl1[:, 0:1], Pb[0][:, :nk], ALU.mult, ALU.add)
            po = psO[par]
            for kb in range(qb + 1):
                pt = psT[kb % 2]; ptS = PT[kb % 2]
                P.transpose(pt[:], Pb[0][:, kb * 128:(kb + 1) * 128], ident)
                P.copy(ptS[:], pt[:], eng="act" if kb % 2 else "dve")
                P.mm(po[:], ptS[:], vs[:, kb, :], start=(kb == 0), stop=(kb == qb))
            o = ob[par]
            P.copy(o[:], po[:], eng="act")
            P.tt(osq[:], o[:], o[:], ALU.mult)
            P.reduce(ss[:], osq[:], ALU.add)
            P.act(ss[:], ss[:], AF.Sqrt, bias=EPS, scale=1.0 / 256)
            P.recip(ss[:], ss[:])
            P.stt(o[:], o[:], ss[:, 0:1], gs[:], ALU.mult, ALU.mult)
            P.ts(o[:], o[:], 1.0 - lambda_init, None, ALU.mult)
            ot = oTs[par]
            for c in range(2):
                pt = psT[c]
                P.transpose(pt[:], o[:, c * 128:(c + 1) * 128], ident)
                P.copy(ot[:, c, :], pt[:], eng="act" if c else "dve")
            P.dma(oal[hd * 256:(hd + 1) * 256, qb * 128:(qb + 1) * 128].rearrange("(c p) t -> p c t", p=128), ot[:])
    P.end()


def build_fused(S, NK):
    T = S // 4
    NE = NK * NK
    NCH = 52
    P = Prog()
    ein = lambda n, s, dt=F32: P.dram(n, s, dt, kind="ExternalInput")
    xin = ein("xin", [D, T]); cT = ein("cT", [128, 16, 2]); adaW = ein("adaW", [D, NCH * 128]); adab = ein("adab", [128, NCH])
    posb = ein("posb", [128, T], I32); invf = ein("invf", [128, 2]); ident_d = ein("ident", [128, 128])
    gconsts = ein("gconsts", [128, 4, 128]); aconsts = ein("aconsts", [128, 2, 128])
    gains = ein("gains", [10, 128, KC])
    gw = []
    for l in range(2):
        gw.append(dict(w_qkvz=ein(f"g{l}_wqkvz", [D, 2048]), w_ab=ein(f"g{l}_wab", [D, 8]), convw=ein(f"g{l}_convw", [128, 12, 4]),
                       hp=ein(f"g{l}_hp", [128, 2, NH]), ogain=ein(f"g{l}_ogain", [128, 1]), g_mix=gains[l]))
    mw = []
    for l in range(4):
        mw.append(dict(w_out=ein(f"m{l}_wout", [D, D]), w_pq=ein(f"m{l}_wpq", [D, D]), keysT=ein(f"m{l}_keysT", [128, 2, NK]),
                       UT=ein(f"m{l}_UT", [D, NE]), Vt=ein(f"m{l}_Vt", [NE, D]), g_ffn=gains[4 + l], final_g=gains[9]))
    kvW = ein("kvW", [D, 4096]); kvWsw = ein("kvWsw", [D, 2048])
    qW = [ein(f"qW{j}", [D, D]) for j in range(2)]; qWsw = [ein(f"qWsw{j}", [D, D]) for j in range(2)]
    lamp = [ein(f"lamp{j}", [128, 4, 128]) for j in range(2)]; gsub = [ein(f"gsub{j}", [128, 256]) for j in range(2)]
    yT = P.dram("yT", [D, T], kind="ExternalOutput")
    XW = P.dram("XW", [8 * D * (T // 2)]); AW = P.dram("AW", [8 * 128 * 2 * NCH])
    adal = P.dram("adal", [128, 2, NCH]); adaloc = P.dram("adaloc", [8, 128, NCH])
    xb = [P.dram(f"xb{i}", [4, D, T]) for i in range(2)]
    xl = [xin] + [P.dram(f"xl{i}", [D, T]) for i in range(1, 4)]
    og = [P.dram(f"og{i}", [512, S]) for i in range(4)]
    ol = [P.dram(f"ol{i}", [4, 512, T]) for i in range(4)]
    kout = P.dram("kout", [2048, T]); vout = P.dram("vout", [4, T, 512])
    qout = [P.dram(f"qout{j}", [2048, T]) for j in range(2)]
    kl = P.dram("kl", [4, 512, T]); vl = P.dram("vlc", [4, T, 512]); ql = [P.dram(f"ql{j}", [4, 512, T]) for j in range(2)]
    try:
        _stages(P, S, T, NK, NCH, locals())
    except StopBuild:
        P.stack = None
    return P


def _stages(P, S, T, NK, NCH, L):
    g = lambda k: L[k]
    xin, cT, adaW, adab, gw, gconsts, gains, qW, qWsw, posb, invf, lamp, gsub, aconsts, mw, yT, ident_d, kvW, kvWsw = [g(k) for k in
        "xin cT adaW adab gw gconsts gains qW qWsw posb invf lamp gsub aconsts mw yT ident_d kvW kvWsw".split()]
    XW, AW, adal, adaloc, xb, xl, og, ol, kout, vout, qout, kl, vl, ql = [g(k) for k in "XW AW adal adaloc xb xl og ol kout vout qout kl vl ql".split()]
    pid = P.pid
    bq = (pid // 4) * 4; hp = pid % 4; q4 = pid % 4; bidx = pid // 4
    H = T // 2

    def ex_cols(src, dst):
        R = src.shape[0]
        return [([8, R, H], src[:, p * H:(p + 1) * H], [(dst[:, :, p * H:(p + 1) * H], lambda v: v[ds(bq, 4)], "sp")]) for p in range(2)]

    def ex_heads(src, dst):
        return [([8, 2048, H], src[:, p * H:(p + 1) * H],
                 [(dst[:, :, p * H:(p + 1) * H], lambda v: v[ds(bq, 4), ds(hp * 512, 512), :], "sp")]) for p in range(2)]

    def ex_o(src, dst):
        return [([8, 256, S], src[p * 256:(p + 1) * 256, :],
                 [(dst[:, p * 256:(p + 1) * 256, :], lambda v: v[ds(bq, 4), :, ds(q4 * T, T)], "act")]) for p in range(2)]

    def ex_v(src, dst):
        return [([8, 4, H, 512], src[:, p * H:(p + 1) * H, :],
                 [(dst[:, p * H:(p + 1) * H, :], lambda v: v[ds(bq, 4), ds(hp, 1)].rearrange("a o t d -> a (o t) d"), "sp")]) for p in range(2)]

    stage_exchange(P, XW, ex_cols(xin, xb[0]))
    stage_ada(P, cT, adaW, adab, adal, None, NCH)
    stage_exchange(P, AW, [([8, 128, 2, NCH], adal,
                            [(adaloc, lambda v: v[:, :, ds(bidx, 1), :].rearrange("j p o c -> j p (o c)"), "pool")])])
    for l in range(4):
        if l < 2:
            stage_gdn(P, S, T, xb[l], adaloc, l, gw[l], og[l], gconsts)
        else:
            j = l - 2
            lambda_init = 0.8 - 0.6 * math.exp(-0.3 * l)
            stage_proj(P, T, xl[l], adaloc, (l, 0, 1, 12, 0, 12), gains[l], qW[j], qWsw[j], 2048, 0, 128.0 ** -0.5, posb, invf, qout[j], None, f"q{j}")
            stage_exchange(P, XW, ex_heads(qout[j], ql[j]))
            stage_attn(P, S, T, ql[j], kl, vl, lamp[j], gsub[j], aconsts, lambda_init, og[l], f"a{j}")
        stage_exchange(P, XW, ex_o(og[l], ol[l]))
        fin = (l == 3)
        stage_mid(P, S, T, NK, xl[l], ol[l], adaloc, l, mw[l], yT if fin else xl[l + 1], None, fin, ident_d)
        if l == 0:
            stage_exchange(P, XW, ex_cols(xl[1], xb[1]))
        if l == 1:
            stage_proj(P, T, xl[2], adaloc, (0, 0, 1, 4, 48, 0), gains[8], kvW, kvWsw, 2048, 2048, 1.0, posb, invf, kout, vout, "kv")
            stage_exchange(P, XW, ex_heads(kout, kl))
            stage_exchange(P, XW, ex_v(vout, vl))


def gdn_consts():
    i = np.arange(128)
    ident = np.eye(128, dtype=np.float32)
    triI = (i[:, None] <= i[None, :]).astype(np.float32)
    triS = (i[:, None] < i[None, :]).astype(np.float32)
    ones = np.ones((128, 128), np.float32)
    return np.ascontiguousarray(np.stack([ident, triI, triS, ones], axis=1))


def attn_consts():
    i = np.arange(128)
    ident = np.eye(128, dtype=np.float32)
    cm = np.where(i[None, :] > i[:, None], -1e30, 0.0).astype(np.float32)
    return np.ascontiguousarray(np.stack([ident, cm], axis=1))


def rope_consts():
    inv = (10000.0 ** (-np.arange(0, 128, 2, dtype=np.float32) / 128)).astype(np.float32)
    invf = np.concatenate([inv, inv])
    sign = np.concatenate([-np.ones(64, np.float32), np.ones(64, np.float32)])
    return np.ascontiguousarray(np.stack([invf, sign], axis=1))


def swap_cols(W, NR):
    Wr = W[:, :NR].reshape(W.shape[0], NR // 128, 2, 64)
    return np.ascontiguousarray(Wr[:, :, ::-1, :].reshape(W.shape[0], NR))


def _col(v):
    return np.ascontiguousarray(np.asarray(v, np.float32).reshape(16, 128).T)


_NC = {}


def kernel(x, c, positions, ada_w, ada_b, norm_mix_g, norm_ffn_g,
           gdn_w_in, gdn_conv_w, gdn_a_log, gdn_dt_bias, gdn_o_gain, gdn_w_out,
           kv_norm_g, kv_ada_w, kv_ada_b, kv_w,
           diff_w_q, diff_lambda, diff_subln_g, diff_w_out,
           peer_w_q, peer_sub_keys, peer_u, peer_v, final_g):
    A = lambda a: np.asarray(a)
    x, c, positions = A(x), A(c), A(positions)
    B, S, _ = x.shape
    T = S // 4
    NK = A(peer_sub_keys).shape[2]
    Wd = 2048
    f32 = np.float32
    key = (S, NK)
    if key not in _NC:
        _NC[key] = build_fused(S, NK).finish()
    nc = _NC[key]
    cT = np.ascontiguousarray(c.T.reshape(16, 128, 2).transpose(1, 0, 2)).astype(f32)
    gains = np.ascontiguousarray(np.stack([_col(A(norm_mix_g)[l]) for l in range(4)] + [_col(A(norm_ffn_g)[l]) for l in range(4)]
                                          + [_col(A(kv_norm_g)), _col(A(final_g))], axis=0))
    shared = dict(cT=cT, invf=rope_consts(), ident=np.eye(128, dtype=f32), gconsts=gdn_consts(), aconsts=attn_consts(), gains=gains)
    for l in range(4):
        shared[f"m{l}_wout"] = np.ascontiguousarray(A(gdn_w_out[l]) if l < 2 else A(diff_w_out[l - 2]))
        shared[f"m{l}_wpq"] = np.ascontiguousarray(A(peer_w_q[l]))
        shared[f"m{l}_keysT"] = np.ascontiguousarray(A(peer_sub_keys[l]).transpose(2, 0, 1))
        shared[f"m{l}_UT"] = np.ascontiguousarray(A(peer_u[l]).T)
        shared[f"m{l}_Vt"] = np.ascontiguousarray(A(peer_v[l]))
    kvw = np.ascontiguousarray(A(kv_w))
    shared["kvW"] = kvw; shared["kvWsw"] = swap_cols(kvw, 2048)
    for j in range(2):
        wq = np.ascontiguousarray(A(diff_w_q[j]))
        shared[f"qW{j}"] = wq; shared[f"qWsw{j}"] = swap_cols(wq, 2048)
        shared[f"lamp{j}"] = np.ascontiguousarray(np.broadcast_to(A(diff_lambda[j])[None], (128, 4, 128))).astype(f32)
        shared[f"gsub{j}"] = np.ascontiguousarray(np.broadcast_to(A(diff_subln_g[j])[None], (128, 256))).astype(f32)
    maps = []
    for core in range(8):
        b, q = core // 4, core % 4
        m = dict(shared)
        m["xin"] = np.ascontiguousarray(x[b, q * T:(q + 1) * T].T)
        m["posb"] = np.ascontiguousarray(np.broadcast_to(positions[b, q * T:(q + 1) * T][None, :], (128, T))).astype(np.int32)
        Wc = np.concatenate([A(ada_w[l])[:, core * 1536:(core + 1) * 1536] for l in range(4)] + [A(kv_ada_w)[:, core * 512:(core + 1) * 512]], axis=1)
        bc = np.concatenate([A(ada_b[l])[core * 1536:(core + 1) * 1536] for l in range(4)] + [A(kv_ada_b)[core * 512:(core + 1) * 512]])
        m["adaW"] = np.ascontiguousarray(Wc); m["adab"] = np.ascontiguousarray(bc.reshape(52, 128).T)
        hg = q
        heads = range(hg * 4, hg * 4 + 4)
        for l in range(2):
            w_in, conv_w = A(gdn_w_in[l]), A(gdn_conv_w[l])
            cols = [w_in[:, kind * Wd + hh * 128: kind * Wd + (hh + 1) * 128] for hh in heads for kind in range(4)]
            m[f"g{l}_wqkvz"] = np.ascontiguousarray(np.concatenate(cols, axis=1))
            m[f"g{l}_wab"] = np.ascontiguousarray(np.concatenate([w_in[:, 4 * Wd + hg * 4: 4 * Wd + hg * 4 + 4],
                                                                   w_in[:, 4 * Wd + 16 + hg * 4: 4 * Wd + 16 + hg * 4 + 4]], axis=1))
            cwl = [conv_w[:, kind * Wd + hh * 128: kind * Wd + (hh + 1) * 128].T for hh in heads for kind in range(3)]
            m[f"g{l}_convw"] = np.ascontiguousarray(np.stack(cwl, axis=1))
            m[f"g{l}_hp"] = np.ascontiguousarray(np.broadcast_to(np.stack([A(gdn_a_log[l])[hg * 4:hg * 4 + 4], A(gdn_dt_bias[l])[hg * 4:hg * 4 + 4]])[None], (128, 2, 4))).astype(f32)
            m[f"g{l}_ogain"] = np.ascontiguousarray(A(gdn_o_gain[l]).reshape(128, 1))
        maps.append(m)
    res = run_bass_kernel_spmd(nc, maps, core_ids=list(range(8))).results
    out = np.empty((B, S, D), f32)
    for core in range(8):
        b, q = core // 4, core % 4
        out[b, q * T:(q + 1) * T] = res[core]["yT"].T
    return out
```
